# Optimizing a Trainium2 kernel written in Bass

```python
import math
import jax, jax.numpy as jnp
from jax import lax
import numpy as np

D_MODEL = 2048
BATCH = 8
SEQ = 2048
DEPTH = 4

MIX_DIM = D_MODEL
RWKV_HEAD = 64
RWKV_DIM = MIX_DIM // 2
RWKV_HEADS = RWKV_DIM // RWKV_HEAD
RWKV_DECAY_LORA = max(32, int(round(1.8 * RWKV_DIM ** 0.5 / 32)) * 32)
RWKV_AAA_LORA = max(32, int(round(1.8 * RWKV_DIM ** 0.5 / 32)) * 32)
RWKV_MV_LORA = max(32, int(round(0.6 * RWKV_DIM ** 0.5 / 32)) * 32)
RWKV_GATE_LORA = max(32, int(round(0.6 * RWKV_DIM ** 0.8 / 32)) * 32)
RWKV_GN_EPS = RWKV_HEAD * 1e-5
RWKV_L2_EPS = 1e-12

GDN_HEAD = 128
GDN_V_DIM = MIX_DIM - RWKV_DIM
GDN_V_HEADS = GDN_V_DIM // GDN_HEAD
GDN_QK_HEADS = GDN_V_HEADS // 2
GDN_QK_DIM = GDN_QK_HEADS * GDN_HEAD
GDN_CONV = 4
GDN_CHUNK = 64
GDN_L2_EPS = 1e-6
GDN_NORM_EPS = 1e-6

D_FF = -(-8 * D_MODEL // (3 * 256)) * 256
NORM_EPS = 1e-5

N_RWKV_IN = 3 * RWKV_DIM + RWKV_DECAY_LORA + RWKV_AAA_LORA + RWKV_GATE_LORA
N_GDN_IN = 2 * GDN_QK_DIM + 2 * GDN_V_DIM + 2 * GDN_V_HEADS
N_IN = N_RWKV_IN + N_GDN_IN

kernel_name = 'hybrid_rwkv7_gdn_trunk'


def split_cols(p, sizes):
    offsets = np.cumsum(np.array(sizes))[:-1].tolist()
    return jnp.split(p, offsets, axis=-1)


def rms_norm(x, w, eps=NORM_EPS):
    x32 = x.astype(jnp.float32)
    y = x32 * lax.rsqrt(jnp.mean(x32 * x32, axis=-1, keepdims=True) + eps)
    return (y * w.astype(jnp.float32)).astype(x.dtype)


def l2_normalize(x, eps):
    x32 = x.astype(jnp.float32)
    return x32 * lax.rsqrt(jnp.sum(x32 * x32, axis=-1, keepdims=True) + eps)


def token_shift_lerp(p, mu):
    prev = jnp.pad(p, ((0, 0), (1, 0), (0, 0)))[:, :-1]
    return p + (prev - p) * mu


def causal_depthwise_conv(x, w):
    K, C = w.shape
    return lax.conv_general_dilated(
        x, w[:, None, :].astype(x.dtype), window_strides=(1,), padding=((K - 1, 0),),
        dimension_numbers=('NWC', 'WIO', 'NWC'), feature_group_count=C)


def rwkv7_recurrence(r, w, k, v, a, b):
    B, T, H, N = r.shape

    def step(S, inp):
        r_t, w_t, k_t, v_t, a_t, b_t = inp
        Sa = jnp.einsum('bhvk,bhk->bhv', S, a_t)
        S = S * w_t[:, :, None, :] + Sa[..., None] * b_t[:, :, None, :] + v_t[..., None] * k_t[:, :, None, :]
        return S, jnp.einsum('bhvk,bhk->bhv', S, r_t)

    xs = tuple(jnp.moveaxis(t, 1, 0) for t in (r, w, k, v, a, b))
    S0 = jnp.zeros((B, H, N, N), jnp.float32)
    _, y = lax.scan(step, S0, xs)
    return jnp.moveaxis(y, 0, 1)


def rwkv7_mixer(p, vres, v_first, mu, w0, w_up, a0, a_up, g_up, k_k, k_a, r_k, ln_w, ln_b):
    B, T, _ = p.shape
    H, N = RWKV_HEADS, RWKV_HEAD
    p = token_shift_lerp(p.astype(jnp.float32), mu)
    r, k, v, wd, ad, gd = split_cols(
        p, (RWKV_DIM, RWKV_DIM, RWKV_DIM, RWKV_DECAY_LORA, RWKV_AAA_LORA, RWKV_GATE_LORA))
    w = -jax.nn.softplus(-(w0 + jnp.tanh(wd) @ w_up)) - 0.5
    decay = jnp.exp(-jnp.exp(w))
    if vres is None:
        v_first = v
    else:
        vd, v_up, v0 = vres
        v = v + (v_first - v) * jax.nn.sigmoid(v0 + vd @ v_up)
    a = jax.nn.sigmoid(a0 + ad @ a_up)
    g = jax.nn.sigmoid(gd) @ g_up
    heads = lambda t: t.reshape(B, T, H, N)
    kk = l2_normalize(heads(k * k_k), RWKV_L2_EPS)
    k = k * (1.0 + (a - 1.0) * k_a)
    r, k, v, a, decay = map(heads, (r, k, v, a, decay))
    y = rwkv7_recurrence(r, decay, k, v, -kk, kk * a)
    mean = jnp.mean(y, axis=-1, keepdims=True)
    var = jnp.mean(jnp.square(y - mean), axis=-1, keepdims=True)
    y = ((y - mean) * lax.rsqrt(var + RWKV_GN_EPS)).reshape(B, T, RWKV_DIM) * ln_w + ln_b
    bonus = (jnp.sum(r * k * r_k, axis=-1, keepdims=True) * v).reshape(B, T, RWKV_DIM)
    return (y + bonus) * g, v_first


def gated_delta_rule_chunked(q, k, v, g, beta):
    B, T, H, dk = q.shape
    dv = v.shape[-1]
    C = GDN_CHUNK
    NC = T // C
    q = l2_normalize(q, GDN_L2_EPS) * (dk ** -0.5)
    k = l2_normalize(k, GDN_L2_EPS)

    def to_chunks(t):
        t = t.reshape((B, NC, C, H) + t.shape[3:])
        return jnp.moveaxis(t, 3, 1)

    q, k, v, g, beta = map(to_chunks, (q, k, v, g, beta))
    g = jnp.cumsum(g, axis=-1)
    idx = jnp.arange(C)
    causal = idx[:, None] >= idx[None, :]
    strict = idx[:, None] > idx[None, :]
    gamma = jnp.exp(jnp.where(causal, g[..., :, None] - g[..., None, :], -jnp.inf))
    k_beta = k * beta[..., None]
    v_beta = v * beta[..., None]
    L = jnp.where(strict, jnp.einsum('bhnid,bhnjd->bhnij', k_beta, k) * gamma, 0.0)
    eye = jnp.eye(C, dtype=L.dtype)
    rhs = jnp.concatenate([v_beta, k_beta * jnp.exp(g)[..., None]], axis=-1)
    sol = lax.linalg.triangular_solve(L + eye, rhs, left_side=True, lower=True, unit_diagonal=True)
    u, w = sol[..., :dv], sol[..., dv:]
    A_qk = jnp.einsum('bhnid,bhnjd->bhnij', q, k) * gamma
    q_g = q * jnp.exp(g)[..., None]
    g_last = g[..., -1]
    k_g = k * jnp.exp(g_last[..., None] - g)[..., None]

    def step(S, inp):
        u_i, w_i, qg_i, kg_i, A_i, gl_i = inp
        v_new = u_i - jnp.einsum('bhck,bhkv->bhcv', w_i, S)
        o = jnp.einsum('bhck,bhkv->bhcv', qg_i, S) + jnp.einsum('bhij,bhjv->bhiv', A_i, v_new)
        S = S * jnp.exp(gl_i)[..., None, None] + jnp.einsum('bhck,bhcv->bhkv', kg_i, v_new)
        return S, o

    xs = tuple(jnp.moveaxis(t, 2, 0) for t in (u, w, q_g, k_g, A_qk, g_last))
    S0 = jnp.zeros((B, H, dk, dv), jnp.float32)
    _, o = lax.scan(step, S0, xs)
    return o.transpose(1, 0, 3, 2, 4).reshape(B, T, H, dv)


def gdn_mixer(p, conv_w, A_log, dt_bias, norm_w):
    B, T, _ = p.shape
    p = p.astype(jnp.float32)
    qkv, z, a, b = split_cols(p, (2 * GDN_QK_DIM + GDN_V_DIM, GDN_V_DIM, GDN_V_HEADS, GDN_V_HEADS))
    qkv = jax.nn.silu(causal_depthwise_conv(qkv, conv_w.astype(jnp.float32)))
    q, k, v = split_cols(qkv, (GDN_QK_DIM, GDN_QK_DIM, GDN_V_DIM))
    rep = GDN_V_HEADS // GDN_QK_HEADS
    q = jnp.repeat(q.reshape(B, T, GDN_QK_HEADS, GDN_HEAD), rep, axis=2)
    k = jnp.repeat(k.reshape(B, T, GDN_QK_HEADS, GDN_HEAD), rep, axis=2)
    v = v.reshape(B, T, GDN_V_HEADS, GDN_HEAD)
    g = -jnp.exp(A_log) * jax.nn.softplus(a + dt_bias)
    beta = jax.nn.sigmoid(b)
    o = gated_delta_rule_chunked(q, k, v, g, beta)
    o = o * lax.rsqrt(jnp.mean(o * o, axis=-1, keepdims=True) + GDN_NORM_EPS) * norm_w
    o = o * jax.nn.silu(z.reshape(B, T, GDN_V_HEADS, GDN_HEAD))
    return o.reshape(B, T, GDN_V_DIM)


def setup_inputs(seed: int = 0) -> dict:
    key = jax.random.key(seed)
    ks = iter(jax.random.split(key, 40))
    L, D, Lv = DEPTH, D_MODEL, DEPTH - 1

    def nrm(shape, scale):
        return jax.random.normal(next(ks), shape, jnp.float32) * scale

    def unif(shape, lo, hi):
        return jax.random.uniform(next(ks), shape, jnp.float32, lo, hi)

    dt = jnp.exp(unif((L, GDN_V_HEADS), math.log(1e-3), math.log(1e-1)))
    return {
        'x': nrm((BATCH, SEQ, D), 1.0),
        'attn_norm_w': 1.0 + nrm((L, D), 0.05),
        'w_in': nrm((L, D, N_IN), D ** -0.5),
        'rwkv_mu': unif((L, N_RWKV_IN), 0.0, 1.0),
        'rwkv_w0': unif((L, RWKV_DIM), -6.0, -0.5),
        'rwkv_w_up': nrm((L, RWKV_DECAY_LORA, RWKV_DIM), 0.1 * RWKV_DECAY_LORA ** -0.5),
        'rwkv_a0': nrm((L, RWKV_DIM), 0.1),
        'rwkv_a_up': nrm((L, RWKV_AAA_LORA, RWKV_DIM), 0.5 * RWKV_AAA_LORA ** -0.5),
        'rwkv_g_up': nrm((L, RWKV_GATE_LORA, RWKV_DIM), RWKV_GATE_LORA ** -0.5),
        'rwkv_k_k': 0.85 + nrm((L, RWKV_DIM), 0.05),
        'rwkv_k_a': 1.0 + nrm((L, RWKV_DIM), 0.05),
        'rwkv_r_k': -0.04 + nrm((L, RWKV_HEADS, RWKV_HEAD), 0.02),
        'rwkv_ln_w': 1.0 + nrm((L, RWKV_DIM), 0.05),
        'rwkv_ln_b': nrm((L, RWKV_DIM), 0.02),
        'vres_down': nrm((Lv, D, RWKV_MV_LORA), D ** -0.5),
        'vres_mu': unif((Lv, RWKV_MV_LORA), 0.0, 1.0),
        'vres_up': nrm((Lv, RWKV_MV_LORA, RWKV_DIM), 0.5 * RWKV_MV_LORA ** -0.5),
        'vres_v0': 1.0 + nrm((Lv, RWKV_DIM), 0.1),
        'gdn_conv_w': nrm((L, GDN_CONV, 2 * GDN_QK_DIM + GDN_V_DIM), 0.5),
        'gdn_A_log': jnp.log(unif((L, GDN_V_HEADS), 1.0, 16.0)),
        'gdn_dt_bias': jnp.log(jnp.expm1(dt)),
        'gdn_norm_w': 1.0 + nrm((L, GDN_HEAD), 0.05),
        'w_out': nrm((L, MIX_DIM, D), MIX_DIM ** -0.5),
        'ffn_norm_w': 1.0 + nrm((L, D), 0.05),
        'ffn_w_gate': nrm((L, D, D_FF), D ** -0.5),
        'ffn_w_up': nrm((L, D, D_FF), D ** -0.5),
        'ffn_w_down': nrm((L, D_FF, D), D_FF ** -0.5),
        'final_norm_w': 1.0 + nrm((D,), 0.05),
    }


def reference(x, attn_norm_w, w_in, rwkv_mu, rwkv_w0, rwkv_w_up, rwkv_a0, rwkv_a_up, rwkv_g_up,
              rwkv_k_k, rwkv_k_a, rwkv_r_k, rwkv_ln_w, rwkv_ln_b, vres_down, vres_mu, vres_up, vres_v0,
              gdn_conv_w, gdn_A_log, gdn_dt_bias, gdn_norm_w, w_out, ffn_norm_w, ffn_w_gate, ffn_w_up,
              ffn_w_down, final_norm_w):
    v_first = None
    for l in range(DEPTH):
        h = rms_norm(x, attn_norm_w[l])
        if l == 0:
            w_cat = w_in[0]
        else:
            w_cat = jnp.concatenate([w_in[l], vres_down[l - 1]], axis=1)
        proj = h @ w_cat
        p_rwkv = proj[..., :N_RWKV_IN]
        p_gdn = proj[..., N_RWKV_IN:N_IN]
        if l == 0:
            vres = None
        else:
            vd = token_shift_lerp(proj[..., N_IN:].astype(jnp.float32), vres_mu[l - 1])
            vres = (vd, vres_up[l - 1], vres_v0[l - 1])
        y_a, v_first = rwkv7_mixer(p_rwkv, vres, v_first, rwkv_mu[l], rwkv_w0[l], rwkv_w_up[l],
                                   rwkv_a0[l], rwkv_a_up[l], rwkv_g_up[l], rwkv_k_k[l], rwkv_k_a[l],
                                   rwkv_r_k[l], rwkv_ln_w[l], rwkv_ln_b[l])
        y_b = gdn_mixer(p_gdn, gdn_conv_w[l], gdn_A_log[l], gdn_dt_bias[l], gdn_norm_w[l])
        y = jnp.concatenate([y_a, y_b], axis=-1).astype(x.dtype)
        x = x + y @ w_out[l]
        h = rms_norm(x, ffn_norm_w[l])
        x = x + (jax.nn.silu(h @ ffn_w_gate[l]) * (h @ ffn_w_up[l])) @ ffn_w_down[l]
    return rms_norm(x, final_norm_w)
```

```python
import numpy as np
from contextlib import ExitStack
import concourse.bass as bass
import concourse.mybir as mybir

F32 = mybir.dt.float32
BF16 = mybir.dt.bfloat16
AF = mybir.ActivationFunctionType
ALU = mybir.AluOpType

SAME_ENGINE_SYNC = True


class V:
    __slots__ = ("tile", "ap")

    def __init__(self, tile, ap):
        self.tile = tile
        self.ap = ap


class Tk:
    def __init__(self, h, name):
        self.h = h
        self.name = name
        self.w = None
        self.r = {}

    def __getitem__(self, k):
        return V(self, self.h[k])

    def sub(self, k, name=None):
        return Tk(self.h[k], name or self.name + "_s")


class QView:
    def __init__(self, tile, ap):
        self.tile = tile
        self.apb = ap

    def __getitem__(self, k):
        return V(self.tile, self.apb[k])


class Prog:
    ENG = ("pe", "act", "dve", "pool", "sp")

    def __init__(self, nc):
        self.nc = nc
        self.ops = {e: [] for e in self.ENG}
        self.dma_cnt = {}
        self.es = ExitStack()

    def sbuf(self, name, shape, dt):
        h = self.es.enter_context(self.nc.sbuf_tensor(name, list(shape), dt))
        n = 1
        for d in shape[1:]:
            n *= d
        self.sbytes = getattr(self, "sbytes", 0) + n * (2 if dt == BF16 else 4)
        return Tk(h, name)

    def psum(self, name, shape, dt):
        h = self.es.enter_context(self.nc.psum_tensor(name, list(shape), dt))
        return Tk(h, name)

    def op(self, eng, fn, R=(), W=(), dma_sem=None):
        deps = []
        for v in R:
            t = v.tile if isinstance(v, V) else v
            if t.w is not None:
                deps.append(t.w)
        for v in W:
            t = v.tile if isinstance(v, V) else v
            if t.w is not None:
                deps.append(t.w)
            deps.extend(t.r.values())
        idx = len(self.ops[eng])
        if dma_sem is not None:
            self.dma_cnt[dma_sem] = self.dma_cnt.get(dma_sem, 0) + 16
            tok = ("D", dma_sem, self.dma_cnt[dma_sem])
        else:
            tok = ("E", eng, idx)
        self.ops[eng].append(dict(fn=fn, deps=deps, tok=tok, dma_sem=dma_sem, sig=False))
        for v in R:
            t = v.tile if isinstance(v, V) else v
            key = tok[:2]
            old = t.r.get(key)
            if old is None or old[2] < tok[2]:
                t.r[key] = tok
        for v in W:
            t = v.tile if isinstance(v, V) else v
            t.w = tok
            t.r = {}
        return tok

    def fence(self, A, B):
        for b in B:
            for a in A:
                toks = list(a.r.values())
                if a.w is not None:
                    toks.append(a.w)
                for tok in toks:
                    key = tok[:2]
                    old = b.r.get(key)
                    if old is None or old[2] < tok[2]:
                        b.r[key] = tok

    def wait_tok(self, eng, tok):
        self.ops[eng].append(dict(fn=None, deps=[tok], tok=("E", eng, len(self.ops[eng])), dma_sem=None, sig=False))

    def mm(self, out, lhsT, rhs, start=True, stop=True, extraR=()):
        self.op("pe", lambda e: e.matmul(out.ap, lhsT.ap, rhs.ap, start=start, stop=stop),
                R=[lhsT, rhs] + list(extraR) + ([] if start else [out]), W=[out])

    def act(self, out, in_, func, bias=None, scale=None, accum_out=None, eng="act"):
        kw = {}
        R = [in_]
        W = [out]
        if bias is not None:
            if isinstance(bias, V):
                kw["bias"] = bias.ap
                R.append(bias)
            else:
                kw["bias"] = bias
        if scale is not None:
            if isinstance(scale, V):
                kw["scale"] = scale.ap
                R.append(scale)
            else:
                kw["scale"] = scale
        if accum_out is not None:
            kw["accum_out"] = accum_out.ap
            W.append(accum_out)
        self.op("act", lambda e: e.activation(out.ap, in_.ap, func, **kw), R=R, W=W)

    def _sc(self, s, R):
        if isinstance(s, V):
            R.append(s)
            return s.ap
        return s

    def ts(self, out, in0, s1, op0, s2=None, op1=None, eng="dve", accum_out=None):
        R = [in0]
        a1 = self._sc(s1, R)
        a2 = self._sc(s2, R) if s2 is not None else None
        W = [out]
        kw = {}
        if accum_out is not None:
            kw["accum_out"] = accum_out.ap
            W.append(accum_out)
        if op1 is None:
            self.op(eng, lambda e: e.tensor_scalar(out.ap, in0.ap, a1, None, op0, **kw), R=R, W=W)
        else:
            self.op(eng, lambda e: e.tensor_scalar(out.ap, in0.ap, a1, a2, op0, op1, **kw), R=R, W=W)

    def tt(self, out, in0, in1, op, eng="dve"):
        self.op(eng, lambda e: e.tensor_tensor(out.ap, in0.ap, in1.ap, op), R=[in0, in1], W=[out])

    def stt(self, out, in0, s, in1, op0, op1):
        R = [in0, in1]
        a = self._sc(s, R)
        self.op("dve", lambda e: e.scalar_tensor_tensor(out.ap, in0.ap, a, in1.ap, op0, op1), R=R, W=[out])

    def copy(self, out, in_, eng="dve"):
        if eng == "act":
            self.op("act", lambda e: e.copy(out.ap, in_.ap), R=[in_], W=[out])
        else:
            self.op(eng, lambda e: e.tensor_copy(out.ap, in_.ap), R=[in_], W=[out])

    def memset(self, out, val, eng="dve"):
        self.op(eng, lambda e: e.memset(out.ap, val), R=[], W=[out])

    def dma(self, q, out, in_, sem, out_is_dram=False, in_is_dram=True):
        R = [] if in_is_dram else [in_]
        W = [] if out_is_dram else [out]
        oap = out if out_is_dram else out.ap
        iap = in_ if in_is_dram else in_.ap
        return self.op(q, lambda e: e.dma_start(out=oap, in_=iap), R=R, W=W, dma_sem=sem)

    def emit(self):
        nc = self.nc
        ops = self.ops
        for e in self.ENG:
            for o in ops[e]:
                nd = []
                for d in o["deps"]:
                    if d[0] == "E":
                        if d[1] == e and (e in ("pe", "sp") or not SAME_ENGINE_SYNC):
                            continue
                        ops[d[1]][d[2]]["sig"] = True
                    nd.append(d)
                o["deps"] = nd
        sigval = {}
        for e in self.ENG:
            c = 0
            for i, o in enumerate(ops[e]):
                if o["sig"]:
                    c += 1
                    sigval[(e, i)] = c
        sems = {}
        for e in self.ENG:
            sems[e] = self.es.enter_context(nc.semaphore("s_" + e))
        for n in self.dma_cnt:
            sems["D" + n] = self.es.enter_context(nc.semaphore("d_" + n))
        engobj = {"pe": "tensor", "act": "scalar", "dve": "vector", "pool": "gpsimd", "sp": "sync"}
        self.stats = {e: len(ops[e]) for e in self.ENG}
        self.nwaits = 0

        def run_engine(ename, eobj):
            seen = {}
            for i, o in enumerate(ops[ename]):
                need = {}
                for d in o["deps"]:
                    if d[0] == "E":
                        s, v = d[1], sigval[(d[1], d[2])]
                    else:
                        s, v = "D" + d[1], d[2]
                    if need.get(s, 0) < v:
                        need[s] = v
                for s, v in need.items():
                    if seen.get(s, 0) >= v:
                        continue
                    eobj.wait_ge(sems[s], v)
                    self.nwaits += 1
                    seen[s] = v
                if o["fn"] is None:
                    continue
                ins = o["fn"](eobj)
                if o["dma_sem"] is not None:
                    ins.then_inc(sems["D" + o["dma_sem"]], 16)
                elif o["sig"]:
                    ins.then_inc(sems[ename], 1)

        with nc.Block() as block:
            for ename in self.ENG:
                deco = getattr(block, engobj[ename])

                def body(eobj, ename=ename):
                    run_engine(ename, eobj)

                deco(body)
        self.es.close()

import numpy as np
from concourse.bass_utils import run_bass_kernel_spmd

D = 2048
T_FULL = 2048
TT = 512
NWB = 2
LIMIT = 99
SUBLIM = 99
NLAYER = 4
DFF = 5632
GB = 3360
BIG = 30000.0

PCOLS = [("anw", 16), ("fnw", 16), ("mu_r", 8), ("mu_k", 8), ("mu_v", 8), ("mu_A", 1), ("mu_B", 1),
         ("mu_C", 1), ("w0", 8), ("a0", 8), ("k_k", 8), ("k_a", 8), ("r_k", 8), ("ln_w", 8),
         ("ln_b", 8), ("v0", 8), ("conv", 64), ("gnw", 1), ("Alog", 8), ("dtb", 8)]
POFF = {}
_o = 0
for _n, _c in PCOLS:
    POFF[_n] = _o
    _o += _c
NPL = _o
NPP = NPL * NLAYER + 16


def block_plan():
    pl = [("lab", 16, 256), ("lc", 16, 144)]
    for p in range(8):
        pl += [("rk%d" % p, 16, 256), ("v%d" % p, 16, 128), ("up%d" % p, 1, 640)]
    for j in range(4):
        pl += [("gqk%d" % j, 16, 256), ("gv%d" % j, 16, 256), ("gz%d" % j, 16, 256)]
    for i in range(8):
        pl += [("out%d" % i, 16, 256)]
    for f in range(44):
        pl += [("gu%d" % f, 16, 256)]
    for m in range(16):
        pl += [("dn%d_0" % m, 22, 128), ("dn%d_1" % m, 22, 128)]
    return pl


PLAN = block_plan()
WPL = sum(k * n for _, k, n in PLAN)


def _pk(blk):
    K = blk.shape[0] // 128
    n = blk.shape[1]
    return blk.reshape(K, 128, n).transpose(1, 0, 2).reshape(128, K * n)


def host_pack_weights(inp, nl):
    out = np.empty((128, WPL * nl), np.float32)
    z = np.zeros
    for l in range(nl):
        Wl = inp["w_in"][l]
        vd = inp["vres_down"][l - 1] if l > 0 else z((D, 32), np.float32)
        blocks = []
        blocks.append(Wl[:, 3072:3328])
        blocks.append(np.concatenate([Wl[:, 3328:3360], vd, z((D, 64), np.float32), Wl[:, 6432:6448]], axis=1))
        w_up = inp["rwkv_w_up"][l]
        a_up = inp["rwkv_a_up"][l]
        g_up = inp["rwkv_g_up"][l]
        v_up = inp["vres_up"][l - 1] if l > 0 else z((32, 1024), np.float32)
        wpad = np.concatenate([w_up, z((64, 1024), np.float32)], axis=0)
        apad = np.concatenate([z((64, 1024), np.float32), a_up], axis=0)
        g1 = g_up[0:128]
        g2 = np.concatenate([g_up[128:160], z((96, 1024), np.float32)], axis=0)
        vpad = np.concatenate([z((32, 1024), np.float32), v_up, z((64, 1024), np.float32)], axis=0)
        for p in range(8):
            s = slice(p * 128, (p + 1) * 128)
            blocks.append(np.concatenate([Wl[:, p * 128:(p + 1) * 128], Wl[:, 1024 + p * 128:1024 + (p + 1) * 128]], axis=1))
            blocks.append(Wl[:, 2048 + p * 128:2048 + (p + 1) * 128])
            blocks.append(np.concatenate([wpad[:, s], apad[:, s], g1[:, s], g2[:, s], vpad[:, s]], axis=1))
        for j in range(4):
            blocks.append(np.concatenate([Wl[:, GB + j * 128:GB + (j + 1) * 128],
                                          Wl[:, GB + 512 + j * 128:GB + 512 + (j + 1) * 128]], axis=1))
            blocks.append(Wl[:, GB + 1024 + 2 * j * 128:GB + 1024 + (2 * j + 2) * 128])
            blocks.append(Wl[:, GB + 2048 + 2 * j * 128:GB + 2048 + (2 * j + 2) * 128])
        Wo = inp["w_out"][l]
        for i in range(8):
            blocks.append(Wo[:, 256 * i:256 * (i + 1)])
        Wg = inp["ffn_w_gate"][l]
        Wu = inp["ffn_w_up"][l]
        for f in range(44):
            blocks.append(np.concatenate([Wg[:, 128 * f:128 * (f + 1)], Wu[:, 128 * f:128 * (f + 1)]], axis=1))
        Wd = inp["ffn_w_down"][l]
        for m in range(16):
            for h in range(2):
                blocks.append(Wd[h * 2816:(h + 1) * 2816, 128 * m:128 * (m + 1)])
        off = l * WPL
        assert len(blocks) == len(PLAN)
        for (nm, K, n), b in zip(PLAN, blocks):
            assert b.shape == (K * 128, n), (nm, b.shape)
            out[:, off:off + K * n] = _pk(np.ascontiguousarray(b, dtype=np.float32))
            off += K * n
        assert off == (l + 1) * WPL
    return out


def host_pack_params(inp, nl):
    pp = np.zeros((128, NPP), np.float32)

    def colv(v, n):
        return np.asarray(v, np.float32).reshape(n, 128).T

    for l in range(nl):
        b = l * NPL

        def put(name, arr):
            pp[:, b + POFF[name]:b + POFF[name] + arr.shape[1]] = arr

        put("anw", colv(inp["attn_norm_w"][l], 16))
        put("fnw", colv(inp["ffn_norm_w"][l], 16))
        mu = np.asarray(inp["rwkv_mu"][l], np.float32)
        put("mu_r", colv(mu[0:1024], 8))
        put("mu_k", colv(mu[1024:2048], 8))
        put("mu_v", colv(mu[2048:3072], 8))
        put("mu_A", colv(mu[3072:3200], 1))
        put("mu_B", colv(mu[3200:3328], 1))
        vm = np.asarray(inp["vres_mu"][l - 1], np.float32) if l > 0 else np.zeros(32, np.float32)
        put("mu_C", colv(np.concatenate([mu[3328:3360], vm, np.zeros(64, np.float32)]), 1))
        put("w0", colv(inp["rwkv_w0"][l], 8))
        put("a0", colv(inp["rwkv_a0"][l], 8))
        put("k_k", colv(inp["rwkv_k_k"][l], 8))
        put("k_a", colv(inp["rwkv_k_a"][l], 8))
        put("r_k", colv(np.asarray(inp["rwkv_r_k"][l]).reshape(-1), 8))
        put("ln_w", colv(inp["rwkv_ln_w"][l], 8))
        put("ln_b", colv(inp["rwkv_ln_b"][l], 8))
        if l > 0:
            put("v0", colv(inp["vres_v0"][l - 1], 8))
        cw = np.asarray(inp["gdn_conv_w"][l], np.float32)
        put("conv", cw.reshape(4, 16, 128).transpose(2, 1, 0).reshape(128, 64))
        put("gnw", colv(inp["gdn_norm_w"][l], 1))
        put("Alog", np.tile(np.asarray(inp["gdn_A_log"][l], np.float32)[None, :], (128, 1)))
        put("dtb", np.tile(np.asarray(inp["gdn_dt_bias"][l], np.float32)[None, :], (128, 1)))
    pp[:, NPL * NLAYER:NPL * NLAYER + 16] = colv(inp["final_norm_w"], 16)
    return pp


def build_program(NT, NL, dbg=None):
    nc = bass.Bass("TRN2", target_bir_lowering=False)
    Ttot = NT * TT
    xT_d = nc.dram_tensor("xT", [D, Ttot], F32, kind="ExternalInput").ap()
    wp_d = nc.dram_tensor("wp", [128, WPL * NL], F32, kind="ExternalInput").ap()
    pp_d = nc.dram_tensor("pp", [128, NPP], F32, kind="ExternalInput").ap()
    oT_d = nc.dram_tensor("oT", [D, Ttot], F32, kind="ExternalOutput").ap()
    dbg_d = {}
    if dbg:
        for nm, shp in dbg.items():
            dbg_d[nm] = nc.dram_tensor("dbg_" + nm, list(shp), F32, kind="ExternalOutput").ap()
    xT_v = xT_d.rearrange("(c p) t -> p c t", p=128)
    oT_v = oT_d.rearrange("(c p) t -> p c t", p=128)

    P = Prog(nc)
    S, PS = P.sbuf, P.psum
    xT = S("xT_sb", [128, 16, TT], F32)
    hT = S("hT", [128, 16, TT], BF16)
    scr = S("scr", [128, 44 * TT], BF16)
    stage = [S("stage%d" % i, [128, 2048], F32) for i in range(2)]
    wb = [S("wb%d" % i, [128, 4096], BF16) for i in range(NWB)]
    pp = S("pp_sb", [128, NPP], F32)
    der = S("der", [128, NLAYER * 24], F32)
    Srw = [S("Srw%d" % l, [128, 8, 128], F32) for l in range(NL)]
    Sgd = [S("Sgd%d" % l, [128, 8, 128], F32) for l in range(NL)]
    vfirst = S("vfirst", [128, 8, TT], BF16)
    carry = [S("carry%d" % l, [128, 27 + 48], F32) for l in range(NL)]
    identb = S("identb", [128, 128], BF16)
    identf = S("identf", [128, 128], F32)
    onesb = S("onesb", [128, 128], BF16)
    onesf = S("onesf", [128, 128], F32)
    bdones = S("bdones", [128, 128], BF16)
    UTs = S("UTs", [128, 128], F32)
    UTi = S("UTi", [128, 128], F32)
    LTs = S("LTs", [128, 128], F32)
    BIGU = S("BIGU", [128, 128], F32)
    NBIGL = S("NBIGL", [128, 128], F32)
    scanmask = S("scanmask", [128, TT], F32)
    pbank = [PS("pb%d" % i, [128, 512], F32) for i in range(8)]
    pdq = {"i": 0}

    def PD():
        t = pbank[pdq["i"] % 5]
        pdq["i"] += 1
        return t

    pquart = []
    for i in range(5, 8):
        for q in range(4):
            pquart.append(QView(pbank[i], pbank[i].h[:, q * 128:(q + 1) * 128]))
    pqq = {"i": 0}

    def PQ():
        t = pquart[pqq["i"] % 12]
        pqq["i"] += 1
        return t

    f32s = [S("f%d" % i, [128, TT + 4], F32) for i in range(12)]
    yT = Tk(scr.h[:, 0:16 * TT].rearrange("p (c t) -> p c t", t=TT), "yT")
    actT = Tk(scr.h[:, 0:44 * TT].rearrange("p (c t) -> p c t", t=TT), "actT")
    _bx = {"o": 16 * TT}
    mixer_tiles = [yT]

    def BX(name, n):
        o = _bx["o"]
        _bx["o"] += n
        assert _bx["o"] <= 44 * TT, name
        t = Tk(scr.h[:, o:o + n], name)
        mixer_tiles.append(t)
        return t

    sqb = BX("sqb", TT)
    actA = BX("actA", TT)
    actB = BX("actB", TT)
    actC = BX("actC", TT)
    bd_a = BX("bd_a", 1024)
    bd_r = BX("bd_r", 1024)
    bd_b = BX("bd_b", 1024)
    bd_k = BX("bd_k", 1024)
    bd_v = BX("bd_v", 1024)
    tr_b = BX("tr_b", 1024)
    tr_k = BX("tr_k", 1024)
    tr_v = BX("tr_v", 1024)
    sm = [BX("sm%d" % i, 128) for i in range(16)]
    qTb = BX("qTb", TT)
    kTb = BX("kTb", TT)
    zs = [BX("zs%d" % i, TT) for i in range(2)]
    ybf = sqb
    abtm = S("abtm", [128, 4, 16], F32)
    gsc = {nm: S("gsc_" + nm, [128, 4, 8], F32) for nm in
           ("g", "beta", "nbeta", "gc", "ngc", "egc", "begc", "ekg", "egl", "tmp", "tmp2")}
    wcs = S("wcs", [128, 8], F32)
    gf = [S("gf%d" % i, [128, 128], F32) for i in range(4)]

    M = mybir.AluOpType
    P.memset(onesf[:, :], 1.0, eng="pool")
    P.memset(identf[:, :], 1.0, eng="pool")
    P.memset(UTs[:, :], 1.0, eng="pool")
    P.memset(UTi[:, :], 1.0, eng="pool")
    P.memset(LTs[:, :], 1.0, eng="pool")

    def asel(t, pat, cmp, base, cm):
        P.op("pool", lambda e: e.affine_select(t.h[:, :], t.h[:, :], pat, cmp, 0.0, base=base, channel_multiplier=cm),
             R=[t], W=[t])

    asel(identf, [[-1, 128]], M.is_equal, 0, 1)
    asel(UTs, [[1, 128]], M.is_ge, -1, -1)
    asel(UTi, [[1, 128]], M.is_ge, 0, -1)
    asel(LTs, [[-1, 128]], M.is_ge, -1, 1)
    P.copy(identb[:, :], identf[:, :])
    P.copy(onesb[:, :], onesf[:, :])
    P.memset(bdones[:, :], 0.0)
    P.memset(bdones[0:64, 0:64], 1.0)
    P.memset(bdones[64:128, 64:128], 1.0)
    P.ts(BIGU[:, :], UTi[:, :], BIG, M.mult)
    P.ts(NBIGL[:, :], LTs[:, :], -BIG, M.mult)
    P.memset(scanmask[:, :], 1.0)
    P.op("dve", lambda e: e.memset(scanmask.h[:, 0:TT:64], 0.0), R=[], W=[scanmask])
    for l in range(NL):
        P.memset(Srw[l][:, :, :], 0.0, eng="pool")
        P.memset(Sgd[l][:, :, :], 0.0, eng="pool")
        P.memset(carry[l][:, :], 0.0, eng="pool")
    for t_ in (bd_a, bd_r, bd_b, bd_k, bd_v):
        P.memset(t_[:, :], 0.0, eng="pool")
    P.dma("sp", pp[:, :], pp_d[:, :], "pp")
    for l in range(NL):
        b = l * NPL
        d = l * 24
        P.ts(der[:, d:d + 8], pp[:, b + POFF["w0"]:b + POFF["w0"] + 8], -1.0, M.mult)
        P.ts(der[:, d + 8:d + 16], pp[:, b + POFF["k_a"]:b + POFF["k_a"] + 8], -1.0, M.mult, 1.0, M.add)
        P.act(der[:, d + 16:d + 24], pp[:, b + POFF["Alog"]:b + POFF["Alog"] + 8], AF.Exp)
        P.ts(der[:, d + 16:d + 24], der[:, d + 16:d + 24], -1.0, M.mult)

    wst = {"off": 0, "unit": 0, "blk": 0, "planidx": 0}

    def next_block(l, name):
        nm, K, n = PLAN[wst["planidx"] % len(PLAN)]
        assert nm == name, (nm, name)
        wst["planidx"] += 1
        tot = K * n
        slot = wb[wst["blk"] % NWB]
        wst["blk"] += 1
        o = 0
        while o < tot:
            u = min(2048, tot - o)
            st = stage[wst["unit"] % 2]
            wst["unit"] += 1
            P.dma("sp", st[:, 0:u], wp_d[:, wst["off"]:wst["off"] + u], "st%d" % ((wst["unit"] - 1) % 2))
            P.copy(slot[:, o:o + u], st[:, 0:u], eng="pool")
            wst["off"] += u
            o += u

        def view(kc, c0, c1):
            return V(slot, slot.h[:, kc * n + c0:kc * n + c1])

        return view

    def dense(view, m, rhs_fn, K=16, ps=None, start=True, stop=True):
        if ps is None:
            ps = PD()
        for kc in range(K):
            P.mm(ps[:, :], view(kc, m * 128, (m + 1) * 128), rhs_fn(kc), start=(start and kc == 0), stop=(stop and kc == K - 1))
        return ps

    def pcol(l, name, i=0):
        o = l * NPL + POFF[name] + i
        return pp[:, o:o + 1]

    def rmsnorm_to_hT(wname, l):
        ps = PD()
        for c in range(16):
            P.act(hT[:, c, :], xT[:, c, :], AF.Square)
        for c in range(16):
            P.mm(ps[:, :], onesb[:, :], hT[:, c, :], start=(c == 0), stop=(c == 15))
        rstd = f32s[11]
        P.act(rstd[:, 0:TT], ps[:, :], AF.Sqrt, bias=1e-5, scale=1.0 / D)
        P.op("dve", lambda e: e.reciprocal(rstd.h[:, 0:TT], rstd.h[:, 0:TT]), R=[rstd], W=[rstd])
        for c in range(16):
            if wname == "finw":
                sc = pp[:, NPL * NLAYER + c:NPL * NLAYER + c + 1]
            else:
                sc = pcol(l, wname, c)
            P.stt(hT[:, c, :], xT[:, c, :], sc, rstd[:, 0:TT], M.mult, M.mult)
        return rstd

    def lerp(ps, l, cidx, mu, raw, dtmp, out):
        P.copy(raw[:, 0:1], carry[l][:, cidx:cidx + 1], eng="pool")
        P.act(raw[:, 1:TT + 1], ps[:, :], AF.Copy)
        P.copy(carry[l][:, cidx:cidx + 1], raw[:, TT:TT + 1], eng="pool")
        P.tt(dtmp[:, 0:TT], raw[:, 0:TT], raw[:, 1:TT + 1], M.subtract)
        P.stt(out, dtmp[:, 0:TT], mu, raw[:, 1:TT + 1], M.mult, M.add)

    def rsqrt_inplace(t, ps, scale, eps):
        P.act(t, ps, AF.Sqrt, bias=eps, scale=scale)
        P.op("dve", lambda e: e.reciprocal(t.ap, t.ap), R=[t], W=[t])

    def neumann(Nm, Bm, levels, bufs):
        X0, X1, n0, n1, b0, b1 = bufs
        P.tt(X0[:, :], Bm[:, :], identb[:, :], M.add)
        X, Xo = X0, X1
        Pn, Pb = Nm, Bm
        nn, bb = [n0, n1], [b0, b1]
        for lv in range(levels):
            q1 = PQ()
            P.mm(q1[:, :], Pb[:, :], Pn[:, :])
            Pn2 = nn[lv % 2]
            P.copy(Pn2[:, :], q1[:, :], eng="act")
            if lv < levels - 1:
                q2 = PQ()
                P.mm(q2[:, :], Pn[:, :], Pb[:, :])
                Pb2 = bb[lv % 2]
                P.copy(Pb2[:, :], q2[:, :], eng="dve")
            else:
                Pb2 = None
            q3 = PQ()
            P.mm(q3[:, :], Pn2[:, :], X[:, :], start=True, stop=False)
            P.mm(q3[:, :], identb[:, :], X[:, :], start=False, stop=True)
            P.copy(Xo[:, :], q3[:, :], eng="dve")
            X, Xo = Xo, X
            Pn, Pb = Pn2, Pb2
        return X

    def dump(name, v):
        if dbg and name in dbg_d:
            tok = P.dma("sp", dbg_d[name], v, "dbg", out_is_dram=True, in_is_dram=False)

    def layer(l, ti):
        d0 = l * 24
        if LIMIT == 0:
            return
        wst["off"] = l * WPL
        wst["planidx"] = 0
        P.fence([actT], mixer_tiles)
        for t_ in (bd_a, bd_r, bd_b, bd_k, bd_v):
            P.memset(t_[:, :], 0.0, eng="pool")
        rmsnorm_to_hT("anw", l)
        hk = lambda kc: hT[:, kc, :]
        vw = next_block(l, "lab")
        psA = dense(vw, 0, hk)
        raw, dtmp = f32s[0], f32s[1]
        lerp(psA, l, 24, pcol(l, "mu_A"), raw, dtmp, dtmp[:, 0:TT])
        P.act(actA[0:64, :], dtmp[0:64, 0:TT], AF.Tanh)
        P.copy(actA[64:128, :], dtmp[64:128, 0:TT], eng="dve")
        psB = dense(vw, 1, hk)
        lerp(psB, l, 25, pcol(l, "mu_B"), raw, dtmp, dtmp[:, 0:TT])
        P.act(actB[:, :], dtmp[:, 0:TT], AF.Sigmoid)
        vw = next_block(l, "lc")
        psC = dense(vw, 0, hk)
        lerp(psC, l, 26, pcol(l, "mu_C"), raw, dtmp, dtmp[:, 0:TT])
        P.act(actC[0:32, :], dtmp[0:32, 0:TT], AF.Sigmoid)
        P.copy(actC[32:64, :], dtmp[32:64, 0:TT], eng="dve")
        P.copy(actC[64:128, :], dtmp[64:128, 0:TT], eng="dve")
        for s4 in range(4):
            q = PQ()
            for kc in range(16):
                P.mm(q[:, 0:16], hT[:, kc, s4 * 128:(s4 + 1) * 128], vw(kc, 128, 144), start=(kc == 0), stop=(kc == 15))
            P.copy(abtm[:, s4, :], q[:, 0:16], eng="act")
        if LIMIT == 1:
            return
        for p in range(8):
            rwkv_pair(l, ti, p, hk)
            if LIMIT == 2:
                return
        if LIMIT == 3:
            return
        gdn_scalars(l)
        if LIMIT == 4:
            return
        for j in range(4):
            gdn_group(l, ti, j, hk)
            if LIMIT == 5:
                return
        if LIMIT == 6:
            return
        for i in range(8):
            vw = next_block(l, "out%d" % i)
            for mm_ in range(2):
                ps = dense(vw, mm_, lambda kc: yT[:, kc, :])
                m = 2 * i + mm_
                P.tt(xT[:, m, :], ps[:, :], xT[:, m, :], M.add)
        P.fence(mixer_tiles, [actT])
        rmsnorm_to_hT("fnw", l)
        for f in range(44):
            vw = next_block(l, "gu%d" % f)
            pg = dense(vw, 0, hk)
            sg = f32s[f % 2]
            P.act(sg[:, 0:TT], pg[:, :], AF.Silu)
            pu = dense(vw, 1, hk)
            P.tt(actT[:, f, :], pu[:, :], sg[:, 0:TT], M.mult)
        for m in range(16):
            ps = PD()
            for h in range(2):
                vw = next_block(l, "dn%d_%d" % (m, h))
                for kk in range(22):
                    P.mm(ps[:, :], vw(kk, 0, 128), actT[:, h * 22 + kk, :], start=(h == 0 and kk == 0), stop=(h == 1 and kk == 21))
            P.tt(xT[:, m, :], ps[:, :], xT[:, m, :], M.add)

    def bdwrite(dst, fn):
        dv = dst.h[:, :].rearrange("p (c s) -> p c s", s=128)
        for h in range(2):
            rows = slice(h * 64, (h + 1) * 64)
            fn(rows, V(dst, dv[rows, :, h * 64:(h + 1) * 64]))

    def c3(t, rows):
        return V(t, t.h[rows, 0:TT].rearrange("p (c s) -> p c s", s=64))

    def rwkv_pair(l, ti, p, hk):
        d0 = l * 24
        raw, dtmp, Rt, Kt, Vt, ewp, cumn, Et, kk, bvec, asig, kmod = f32s[0:12]
        vw = next_block(l, "rk%d" % p)
        ps = dense(vw, 0, hk)
        lerp(ps, l, p, pcol(l, "mu_r", p), raw, dtmp, Rt[:, 0:TT])
        ps = dense(vw, 1, hk)
        lerp(ps, l, 8 + p, pcol(l, "mu_k", p), raw, dtmp, Kt[:, 0:TT])
        vw = next_block(l, "v%d" % p)
        ps = dense(vw, 0, hk)
        lerp(ps, l, 16 + p, pcol(l, "mu_v", p), raw, dtmp, Vt[:, 0:TT])
        up = next_block(l, "up%d" % p)
        pw = PD()
        P.mm(pw[:, :], up(0, 0, 128), actA[:, :])
        P.act(ewp[:, 0:TT], pw[:, :], AF.Exp, bias=der[:, d0 + p:d0 + p + 1], scale=-1.0)
        P.act(ewp[:, 0:TT], ewp[:, 0:TT], AF.Ln, bias=1.0)
        P.act(ewp[:, 0:TT], ewp[:, 0:TT], AF.Exp, bias=-0.5, scale=-1.0)
        P.op("dve", lambda e: e.tensor_tensor_scan(cumn.h[:, 0:TT], scanmask.h[:, :], ewp.h[:, 0:TT], 0.0, M.mult, M.add),
             R=[scanmask, ewp], W=[cumn])
        P.act(wcs[:, :], cumn[:, 63:TT:64], AF.Exp, scale=-1.0)
        if SUBLIM == 1:
            return
        pa = PD()
        P.mm(pa[:, :], up(0, 128, 256), actA[:, :])
        P.act(asig[:, 0:TT], pa[:, :], AF.Sigmoid, bias=pcol(l, "a0", p))
        P.ts(kk[:, 0:TT], Kt[:, 0:TT], pcol(l, "k_k", p), M.mult)
        P.act(sqb[:, :], kk[:, 0:TT], AF.Square)
        pss = PD()
        P.mm(pss[:, :], bdones[:, :], sqb[:, :])
        rsqrt_inplace(bvec[:, 0:TT], pss[:, :], 1.0, 1e-12)
        P.tt(kk[:, 0:TT], kk[:, 0:TT], bvec[:, 0:TT], M.mult)
        P.tt(bvec[:, 0:TT], kk[:, 0:TT], asig[:, 0:TT], M.mult)
        P.ts(kmod[:, 0:TT], asig[:, 0:TT], pcol(l, "k_a", p), M.mult, der[:, d0 + 8 + p:d0 + 9 + p], M.add)
        P.tt(kmod[:, 0:TT], kmod[:, 0:TT], Kt[:, 0:TT], M.mult)
        if l == 0:
            P.copy(vfirst[:, p, :], Vt[:, 0:TT], eng="act")
        else:
            pv = PD()
            P.mm(pv[:, :], up(0, 512, 640), actC[:, :])
            P.act(dtmp[:, 0:TT], pv[:, :], AF.Sigmoid, bias=pcol(l, "v0", p))
            P.copy(raw[:, 0:TT], vfirst[:, p, :], eng="act")
            P.tt(raw[:, 0:TT], raw[:, 0:TT], Vt[:, 0:TT], M.subtract)
            P.tt(raw[:, 0:TT], raw[:, 0:TT], dtmp[:, 0:TT], M.mult)
            P.tt(Vt[:, 0:TT], Vt[:, 0:TT], raw[:, 0:TT], M.add)
        pg = PD()
        P.mm(pg[:, :], up(0, 256, 384), actB[:, :], start=True, stop=False)
        P.mm(pg[:, :], up(0, 384, 512), actC[:, :], start=False, stop=True)
        gate = Kt
        P.act(gate[:, 0:TT], pg[:, :], AF.Copy)
        P.stt(sqb[:, :], Rt[:, 0:TT], pcol(l, "r_k", p), kmod[:, 0:TT], M.mult, M.mult)
        pbn = PD()
        P.mm(pbn[:, :], bdones[:, :], sqb[:, :])
        bonus = asig
        P.tt(bonus[:, 0:TT], pbn[:, :], Vt[:, 0:TT], M.mult)
        if SUBLIM == 2:
            return
        P.tt(Et[:, 0:TT], ewp[:, 0:TT], cumn[:, 0:TT], M.subtract)
        P.act(Et[:, 0:TT], Et[:, 0:TT], AF.Exp)
        bdwrite(bd_a, lambda rows, o: P.stt(o, c3(kk, rows), -1.0, c3(Et, rows), M.mult, M.mult))
        P.act(Et[:, 0:TT], cumn[:, 0:TT], AF.Exp, scale=-1.0)
        bdwrite(bd_r, lambda rows, o: P.tt(o, c3(Rt, rows), c3(Et, rows), M.mult))
        P.act(Et[:, 0:TT], cumn[:, 0:TT], AF.Exp)
        bdwrite(bd_b, lambda rows, o: P.tt(o, c3(bvec, rows), c3(Et, rows), M.mult))
        bdwrite(bd_k, lambda rows, o: P.tt(o, c3(kmod, rows), c3(Et, rows), M.mult))
        bdwrite(bd_v, lambda rows, o: P.copy(o, c3(Vt, rows), eng="act"))
        if SUBLIM == 3:
            return
        yout = raw
        S_ = Srw[l]
        Sb = sm[15]
        for c in range(8):
            cs = slice(c * 128, (c + 1) * 128)
            A_, R_, B_, K_, V_ = (V(t, t.h[:, cs]) for t in (bd_a, bd_r, bd_b, bd_k, bd_v))
            for src, dst in ((B_, tr_b), (K_, tr_k), (V_, tr_v)):
                q = PQ()
                P.mm(q[:, :], src, identb[:, :])
                P.copy(dst[:, cs], q[:, :], eng="act")
            Nm, Bm, AkT, ArbT, ArkT = sm[0:5]
            for lhs, rhs, msk, dst in ((A_, B_, LTs, Nm), (B_, A_, UTs, Bm), (K_, A_, UTs, AkT), (B_, R_, UTi, ArbT), (K_, R_, UTi, ArkT)):
                q = PQ()
                P.mm(q[:, :], lhs, rhs)
                P.tt(dst[:, :], q[:, :], msk[:, :], M.mult)
            if SUBLIM == 4:
                return
            X = neumann(Nm, Bm, 5, sm[5:11])
            if SUBLIM == 5:
                return
            P.copy(Sb[:, :], S_[:, p, :], eng="act")
            q = PQ()
            P.mm(q[:, :], A_, Sb[:, :], start=True, stop=False)
            P.mm(q[:, :], AkT[:, :], tr_v[:, cs], start=False, stop=True)
            RHS = sm[11]
            P.copy(RHS[:, :], q[:, :], eng="act")
            q = PQ()
            P.mm(q[:, :], X[:, :], RHS[:, :])
            U = sm[12]
            P.copy(U[:, :], q[:, :], eng="dve")
            q = PQ()
            P.mm(q[:, :], Sb[:, :], R_, start=True, stop=False)
            P.mm(q[:, :], U[:, :], ArbT[:, :], start=False, stop=False)
            P.mm(q[:, :], tr_v[:, cs], ArkT[:, :], start=False, stop=True)
            P.copy(yout[0:64, c * 64:(c + 1) * 64], q[0:64, 0:64], eng="act")
            P.copy(yout[64:128, c * 64:(c + 1) * 64], q[64:128, 64:128], eng="dve")
            q = PQ()
            P.mm(q[:, :], tr_b[:, cs], U[:, :], start=True, stop=False)
            P.mm(q[:, :], tr_k[:, cs], tr_v[:, cs], start=False, stop=True)
            P.tt(S_[:, p, :], q[:, :], S_[:, p, :], M.add)
            P.ts(S_[:, p, :], S_[:, p, :], wcs[:, c:c + 1], M.mult)
            if SUBLIM == 6:
                return
        P.copy(ybf[:, :], yout[:, 0:TT], eng="act")
        pm = PD()
        P.mm(pm[:, :], bdones[:, :], ybf[:, :])
        yc = dtmp
        P.stt(yc[:, 0:TT], pm[:, :], -1.0 / 64, yout[:, 0:TT], M.mult, M.add)
        P.act(sqb[:, :], yc[:, 0:TT], AF.Square)
        pv2 = PD()
        P.mm(pv2[:, :], bdones[:, :], sqb[:, :])
        rsqrt_inplace(ewp[:, 0:TT], pv2[:, :], 1.0 / 64, 64e-5)
        P.tt(yc[:, 0:TT], yc[:, 0:TT], ewp[:, 0:TT], M.mult)
        P.ts(yc[:, 0:TT], yc[:, 0:TT], pcol(l, "ln_w", p), M.mult, pcol(l, "ln_b", p), M.add)
        P.tt(yc[:, 0:TT], yc[:, 0:TT], bonus[:, 0:TT], M.add)
        P.tt(yT[:, p, :], yc[:, 0:TT], gate[:, 0:TT], M.mult)

    def gdn_scalars(l):
        d0 = l * 24
        b = l * NPL
        G = gsc
        for c in range(4):
            P.tt(G["tmp"][:, c, :], abtm[:, c, 0:8], pp[:, b + POFF["dtb"]:b + POFF["dtb"] + 8], M.add)
            P.act(G["tmp"][:, c, :], G["tmp"][:, c, :], AF.Exp)
            P.act(G["tmp"][:, c, :], G["tmp"][:, c, :], AF.Ln, bias=1.0)
            P.tt(G["g"][:, c, :], G["tmp"][:, c, :], der[:, d0 + 16:d0 + 24], M.mult)
            P.act(G["beta"][:, c, :], abtm[:, c, 8:16], AF.Sigmoid)
            P.ts(G["nbeta"][:, c, :], G["beta"][:, c, :], -1.0, M.mult)
            q = PQ()
            P.mm(q[:, 0:8], UTi[:, :], G["g"][:, c, :])
            P.copy(G["gc"][:, c, :], q[:, 0:8], eng="act")
            P.ts(G["ngc"][:, c, :], G["gc"][:, c, :], -1.0, M.mult)
            P.act(G["egc"][:, c, :], G["gc"][:, c, :], AF.Exp)
            P.tt(G["begc"][:, c, :], G["egc"][:, c, :], G["beta"][:, c, :], M.mult)
            q2 = PQ()
            P.mm(q2[:, 0:8], onesf[:, :], G["g"][:, c, :])
            P.act(G["egl"][:, c, :], q2[:, 0:8], AF.Exp)
            P.tt(G["tmp2"][:, c, :], q2[:, 0:8], G["gc"][:, c, :], M.subtract)
            P.act(G["ekg"][:, c, :], G["tmp2"][:, c, :], AF.Exp)

    def conv_silu(ps, l, cc, raw, acc, out, silu=True):
        co = 27 + cc * 3
        P.copy(raw[:, 0:3], carry[l][:, co:co + 3], eng="pool")
        P.act(raw[:, 3:TT + 3], ps[:, :], AF.Copy)
        P.copy(carry[l][:, co:co + 3], raw[:, TT:TT + 3], eng="pool")
        w = lambda tap: pcol(l, "conv", cc * 4 + tap)
        P.ts(acc[:, 0:TT], raw[:, 0:TT], w(0), M.mult)
        for tap in (1, 2, 3):
            P.stt(acc[:, 0:TT], raw[:, tap:TT + tap], w(tap), acc[:, 0:TT], M.mult, M.add)
        P.act(out, acc[:, 0:TT], AF.Silu)

    def gdn_group(l, ti, j, hk):
        G = gsc
        raw, acc, qf, kf, v0f, v1f, o0, o1, rn, t9 = f32s[0:10]
        zsf = [f32s[10], f32s[11]]
        vw = next_block(l, "gqk%d" % j)
        ps = dense(vw, 0, hk)
        conv_silu(ps, l, j, raw, acc, qf[:, 0:TT])
        ps = dense(vw, 1, hk)
        conv_silu(ps, l, 4 + j, raw, acc, kf[:, 0:TT])
        vw = next_block(l, "gv%d" % j)
        vf = [v0f, v1f]
        for hh in range(2):
            ps = dense(vw, hh, hk)
            conv_silu(ps, l, 8 + 2 * j + hh, raw, acc, vf[hh][:, 0:TT])
        vw = next_block(l, "gz%d" % j)
        for hh in range(2):
            ps = dense(vw, hh, hk)
            P.act(zsf[hh][:, 0:TT], ps[:, :], AF.Silu)
        for src, dst, sc in ((qf, qTb, 128.0 ** -0.5), (kf, kTb, 1.0)):
            P.act(sqb[:, :], src[:, 0:TT], AF.Square)
            pss = PD()
            P.mm(pss[:, :], onesb[:, :], sqb[:, :])
            rsqrt_inplace(rn[:, 0:TT], pss[:, :], 1.0, 1e-6)
            P.stt(src[:, 0:TT], src[:, 0:TT], sc, rn[:, 0:TT], M.mult, M.mult)
            P.copy(dst[:, :], src[:, 0:TT], eng="act")
        vTb = [tr_b, tr_k]
        for hh in range(2):
            P.copy(vTb[hh][:, 0:TT], vf[hh][:, 0:TT], eng="act")
        oT = [o0, o1]
        for c in range(4):
            cs = slice(c * 128, (c + 1) * 128)
            qkk = PQ()
            P.mm(qkk[:, :], kTb[:, cs], kTb[:, cs])
            KK = gf[0]
            P.copy(KK[:, :], qkk[:, :], eng="act")
            qqk = PQ()
            P.mm(qqk[:, :], kTb[:, cs], qTb[:, cs])
            QKT = gf[1]
            P.copy(QKT[:, :], qqk[:, :], eng="dve")
            qkt = PQ()
            P.mm(qkt[:, :], kTb[:, cs], identb[:, :])
            ktm = gf[2]
            P.copy(ktm[:, :], qkt[:, :], eng="act")
            for hh in range(2):
                h = 2 * j + hh
                col = lambda nm: G[nm][:, c, h:h + 1]
                gbc = gf[3]
                P.ts(gbc[:, :], onesf[:, :], col("g"), M.mult)
                q1 = PQ()
                P.mm(q1[:, :], gbc[:, :], UTi[:, :], start=True, stop=False)
                P.mm(q1[:, :], identf[:, :], BIGU[:, :], start=False, stop=True)
                Gs = t9
                P.act(Gs[:, 0:128], q1[:, :], AF.Exp, bias=col("gc"), scale=-1.0)
                Nm, Bm, AqkT, bv, kbg, kg = sm[0], sm[1], sm[2], sm[3], sm[4], sm[13]
                P.stt(Nm[:, :], Gs[:, 0:128], col("nbeta"), KK[:, :], M.mult, M.mult)
                q2 = PQ()
                P.mm(q2[:, :], gbc[:, :], UTi[:, :], start=True, stop=False)
                P.mm(q2[:, :], identf[:, :], NBIGL[:, :], start=False, stop=True)
                P.act(Gs[:, 128:256], q2[:, :], AF.Exp, bias=col("ngc"), scale=1.0)
                P.tt(AqkT[:, :], QKT[:, :], Gs[:, 128:256], M.mult)
                q3 = PQ()
                P.mm(q3[:, :], Nm[:, :], identb[:, :])
                P.copy(Bm[:, :], q3[:, :], eng="act")
                q4 = PQ()
                P.mm(q4[:, :], vTb[hh][:, cs], identb[:, :])
                P.ts(bv[:, :], q4[:, :], col("beta"), M.mult)
                P.ts(kbg[:, :], ktm[:, :], col("begc"), M.mult)
                P.ts(kg[:, :], ktm[:, :], col("ekg"), M.mult)
                X = neumann(Nm, Bm, 6, sm[5:11])
                q5 = PQ()
                P.mm(q5[:, :], kbg[:, :], X[:, :])
                wTn = sm[11]
                P.act(wTn[:, :], q5[:, :], AF.Copy, scale=-1.0)
                q6 = PQ()
                P.mm(q6[:, :], gbc[:, :], UTi[:, :])
                P.act(Gs[:, 256:384], q6[:, :], AF.Exp)
                qgT = sm[12]
                P.tt(qgT[:, :], qf[:, cs], Gs[:, 256:384], M.mult)
                S_ = Sgd[l]
                Sb = sm[15]
                P.copy(Sb[:, :], S_[:, h, :], eng="act")
                q7 = PQ()
                P.mm(q7[:, :], X[:, :], bv[:, :], start=True, stop=False)
                P.mm(q7[:, :], wTn[:, :], Sb[:, :], start=False, stop=True)
                vnew = sm[14]
                P.copy(vnew[:, :], q7[:, :], eng="dve")
                q8 = PQ()
                P.mm(q8[:, :], Sb[:, :], qgT[:, :], start=True, stop=False)
                P.mm(q8[:, :], vnew[:, :], AqkT[:, :], start=False, stop=True)
                P.copy(oT[hh][:, cs], q8[:, :], eng="act")
                q9 = PQ()
                P.mm(q9[:, :], kg[:, :], vnew[:, :])
                P.stt(S_[:, h, :], S_[:, h, :], col("egl"), q9[:, :], M.mult, M.add)
        for hh in range(2):
            h = 2 * j + hh
            P.act(sqb[:, :], oT[hh][:, 0:TT], AF.Square)
            pss = PD()
            P.mm(pss[:, :], onesb[:, :], sqb[:, :])
            rsqrt_inplace(rn[:, 0:TT], pss[:, :], 1.0 / 128, 1e-6)
            P.stt(oT[hh][:, 0:TT], oT[hh][:, 0:TT], pcol(l, "gnw"), rn[:, 0:TT], M.mult, M.mult)
            P.tt(yT[:, 8 + h, :], oT[hh][:, 0:TT], zsf[hh][:, 0:TT], M.mult)

    last_tok = None
    for ti in range(NT):
        ts_ = slice(ti * TT, (ti + 1) * TT)
        for c4 in range(4):
            P.dma("sp", xT[:, c4 * 4:(c4 + 1) * 4, :], xT_v[:, c4 * 4:(c4 + 1) * 4, ts_], "xin")
        for l in range(NL):
            layer(l, ti)
        ps = PD()
        for c in range(16):
            P.act(hT[:, c, :], xT[:, c, :], AF.Square)
        for c in range(16):
            P.mm(ps[:, :], onesb[:, :], hT[:, c, :], start=(c == 0), stop=(c == 15))
        rs = f32s[11]
        rsqrt_inplace(rs[:, 0:TT], ps[:, :], 1.0 / D, 1e-5)
        for c in range(16):
            o = NPL * NLAYER + c
            P.stt(xT[:, c, :], xT[:, c, :], pp[:, o:o + 1], rs[:, 0:TT], M.mult, M.mult)
        for c4 in range(4):
            last_tok = P.dma("sp", oT_v[:, c4 * 4:(c4 + 1) * 4, ts_], xT[:, c4 * 4:(c4 + 1) * 4, :], "xout", out_is_dram=True, in_is_dram=False)
    P.wait_tok("sp", last_tok)
    P.emit()
    return nc, P


_CACHE = {}


def run_cores(inp, NT, NL, ncores):
    wp = host_pack_weights(inp, NL)
    ppk = host_pack_params(inp, NL)
    x = np.asarray(inp["x"], np.float32)
    in_maps = []
    for b in range(ncores):
        in_maps.append({"xT": np.ascontiguousarray(x[b, :NT * TT, :].T), "wp": wp, "pp": ppk})
    nc, P = build_program(NT, NL)
    res = run_bass_kernel_spmd(nc, in_maps, core_ids=list(range(ncores)))
    outs = [np.ascontiguousarray(res.results[b]["oT"].T) for b in range(ncores)]
    return np.stack(outs, axis=0)


def kernel(**inputs):
    inp = {k: np.asarray(v) for k, v in inputs.items()}
    out = run_cores(inp, T_FULL // TT, NLAYER, 8)
    return out.astype(np.float32)
```

```python
import numpy as np
from contextlib import ExitStack
import concourse.bass as bass
import concourse.mybir as mybir

F32 = mybir.dt.float32
BF16 = mybir.dt.bfloat16
AF = mybir.ActivationFunctionType
ALU = mybir.AluOpType

SAME_ENGINE_SYNC = True


class V:
    __slots__ = ("tile", "ap")

    def __init__(self, tile, ap):
        self.tile = tile
        self.ap = ap


class Tk:
    def __init__(self, h, name):
        self.h = h
        self.name = name
        self.w = None
        self.r = {}

    def __getitem__(self, k):
        return V(self, self.h[k])

    def sub(self, k, name=None):
        return Tk(self.h[k], name or self.name + "_s")


class QView:
    def __init__(self, tile, ap):
        self.tile = tile
        self.apb = ap

    def __getitem__(self, k):
        return V(self.tile, self.apb[k])


class Prog:
    ENG = ("pe", "act", "dve", "pool", "sp")

    def __init__(self, nc):
        self.nc = nc
        self.ops = {e: [] for e in self.ENG}
        self.dma_cnt = {}
        self.es = ExitStack()

    def sbuf(self, name, shape, dt):
        h = self.es.enter_context(self.nc.sbuf_tensor(name, list(shape), dt))
        n = 1
        for d in shape[1:]:
            n *= d
        self.sbytes = getattr(self, "sbytes", 0) + n * (2 if dt == BF16 else 4)
        return Tk(h, name)

    def psum(self, name, shape, dt):
        h = self.es.enter_context(self.nc.psum_tensor(name, list(shape), dt))
        return Tk(h, name)

    def op(self, eng, fn, R=(), W=(), dma_sem=None):
        deps = []
        for v in R:
            t = v.tile if isinstance(v, V) else v
            if t.w is not None:
                deps.append(t.w)
        for v in W:
            t = v.tile if isinstance(v, V) else v
            if t.w is not None:
                deps.append(t.w)
            deps.extend(t.r.values())
        idx = len(self.ops[eng])
        if dma_sem is not None:
            self.dma_cnt[dma_sem] = self.dma_cnt.get(dma_sem, 0) + 16
            tok = ("D", dma_sem, self.dma_cnt[dma_sem])
        else:
            tok = ("E", eng, idx)
        self.ops[eng].append(dict(fn=fn, deps=deps, tok=tok, dma_sem=dma_sem, sig=False))
        for v in R:
            t = v.tile if isinstance(v, V) else v
            key = tok[:2]
            old = t.r.get(key)
            if old is None or old[2] < tok[2]:
                t.r[key] = tok
        for v in W:
            t = v.tile if isinstance(v, V) else v
            t.w = tok
            t.r = {}
        return tok

    def fence(self, A, B):
        for b in B:
            for a in A:
                toks = list(a.r.values())
                if a.w is not None:
                    toks.append(a.w)
                for tok in toks:
                    key = tok[:2]
                    old = b.r.get(key)
                    if old is None or old[2] < tok[2]:
                        b.r[key] = tok

    def wait_tok(self, eng, tok):
        self.ops[eng].append(dict(fn=None, deps=[tok], tok=("E", eng, len(self.ops[eng])), dma_sem=None, sig=False))

    def mm(self, out, lhsT, rhs, start=True, stop=True, extraR=()):
        self.op("pe", lambda e: e.matmul(out.ap, lhsT.ap, rhs.ap, start=start, stop=stop),
                R=[lhsT, rhs] + list(extraR) + ([] if start else [out]), W=[out])

    def act(self, out, in_, func, bias=None, scale=None, accum_out=None, eng="act"):
        kw = {}
        R = [in_]
        W = [out]
        if bias is not None:
            if isinstance(bias, V):
                kw["bias"] = bias.ap
                R.append(bias)
            else:
                kw["bias"] = bias
        if scale is not None:
            if isinstance(scale, V):
                kw["scale"] = scale.ap
                R.append(scale)
            else:
                kw["scale"] = scale
        if accum_out is not None:
            kw["accum_out"] = accum_out.ap
            W.append(accum_out)
        self.op("act", lambda e: e.activation(out.ap, in_.ap, func, **kw), R=R, W=W)

    def _sc(self, s, R):
        if isinstance(s, V):
            R.append(s)
            return s.ap
        return s

    def ts(self, out, in0, s1, op0, s2=None, op1=None, eng="dve", accum_out=None):
        R = [in0]
        a1 = self._sc(s1, R)
        a2 = self._sc(s2, R) if s2 is not None else None
        W = [out]
        kw = {}
        if accum_out is not None:
            kw["accum_out"] = accum_out.ap
            W.append(accum_out)
        if op1 is None:
            self.op(eng, lambda e: e.tensor_scalar(out.ap, in0.ap, a1, None, op0, **kw), R=R, W=W)
        else:
            self.op(eng, lambda e: e.tensor_scalar(out.ap, in0.ap, a1, a2, op0, op1, **kw), R=R, W=W)

    def tt(self, out, in0, in1, op, eng="dve"):
        self.op(eng, lambda e: e.tensor_tensor(out.ap, in0.ap, in1.ap, op), R=[in0, in1], W=[out])

    def stt(self, out, in0, s, in1, op0, op1):
        R = [in0, in1]
        a = self._sc(s, R)
        self.op("dve", lambda e: e.scalar_tensor_tensor(out.ap, in0.ap, a, in1.ap, op0, op1), R=R, W=[out])

    def copy(self, out, in_, eng="dve"):
        if eng == "act":
            self.op("act", lambda e: e.copy(out.ap, in_.ap), R=[in_], W=[out])
        else:
            self.op(eng, lambda e: e.tensor_copy(out.ap, in_.ap), R=[in_], W=[out])

    def memset(self, out, val, eng="dve"):
        self.op(eng, lambda e: e.memset(out.ap, val), R=[], W=[out])

    def dma(self, q, out, in_, sem, out_is_dram=False, in_is_dram=True):
        R = [] if in_is_dram else [in_]
        W = [] if out_is_dram else [out]
        oap = out if out_is_dram else out.ap
        iap = in_ if in_is_dram else in_.ap
        return self.op(q, lambda e: e.dma_start(out=oap, in_=iap), R=R, W=W, dma_sem=sem)

    def emit(self):
        nc = self.nc
        ops = self.ops
        for e in self.ENG:
            for o in ops[e]:
                nd = []
                for d in o["deps"]:
                    if d[0] == "E":
                        if d[1] == e and (e in ("pe", "sp") or not SAME_ENGINE_SYNC):
                            continue
                        ops[d[1]][d[2]]["sig"] = True
                    nd.append(d)
                o["deps"] = nd
        sigval = {}
        for e in self.ENG:
            c = 0
            for i, o in enumerate(ops[e]):
                if o["sig"]:
                    c += 1
                    sigval[(e, i)] = c
        sems = {}
        for e in self.ENG:
            sems[e] = self.es.enter_context(nc.semaphore("s_" + e))
        for n in self.dma_cnt:
            sems["D" + n] = self.es.enter_context(nc.semaphore("d_" + n))
        engobj = {"pe": "tensor", "act": "scalar", "dve": "vector", "pool": "gpsimd", "sp": "sync"}
        self.stats = {e: len(ops[e]) for e in self.ENG}
        self.nwaits = 0

        def run_engine(ename, eobj):
            seen = {}
            for i, o in enumerate(ops[ename]):
                need = {}
                for d in o["deps"]:
                    if d[0] == "E":
                        s, v = d[1], sigval[(d[1], d[2])]
                    else:
                        s, v = "D" + d[1], d[2]
                    if need.get(s, 0) < v:
                        need[s] = v
                for s, v in need.items():
                    if seen.get(s, 0) >= v:
                        continue
                    eobj.wait_ge(sems[s], v)
                    self.nwaits += 1
                    seen[s] = v
                if o["fn"] is None:
                    continue
                ins = o["fn"](eobj)
                if o["dma_sem"] is not None:
                    ins.then_inc(sems["D" + o["dma_sem"]], 16)
                elif o["sig"]:
                    ins.then_inc(sems[ename], 1)

        with nc.Block() as block:
            for ename in self.ENG:
                deco = getattr(block, engobj[ename])

                def body(eobj, ename=ename):
                    run_engine(ename, eobj)

                deco(body)
        self.es.close()

import numpy as np
from concourse.bass_utils import run_bass_kernel_spmd

D = 2048
T_FULL = 2048
TT = 512
NWB = 3
UNIT = 1024
DMACAST = True
SCRN = 16 * 512 + 18944
LIMIT = 99
SUBLIM = 99
NLAYER = 4
DFF = 5632
GB = 3360
BIG = 30000.0

PCOLS = [("anw", 16), ("fnw", 16), ("mu_r", 8), ("mu_k", 8), ("mu_v", 8), ("mu_A", 1), ("mu_B", 1),
         ("mu_C", 1), ("w0", 8), ("a0", 8), ("k_k", 8), ("k_a", 8), ("r_k", 8), ("ln_w", 8),
         ("ln_b", 8), ("v0", 8), ("conv", 64), ("gnw", 1), ("Alog", 8), ("dtb", 8)]
POFF = {}
_o = 0
for _n, _c in PCOLS:
    POFF[_n] = _o
    _o += _c
NPL = _o
NPP = NPL * NLAYER + 16


def block_plan():
    pl = [("lab", 16, 256), ("lc", 16, 144)]
    for p in range(8):
        pl += [("rk%d" % p, 16, 256), ("v%d" % p, 16, 128), ("up%d" % p, 1, 640)]
    for j in range(4):
        pl += [("gqk%d" % j, 16, 256), ("gv%d" % j, 16, 256), ("gz%d" % j, 16, 256)]
    for i in range(8):
        pl += [("out%d" % i, 16, 256)]
    for f in range(44):
        pl += [("gu%d" % f, 16, 256)]
    for m in range(16):
        pl += [("dn%d_0" % m, 22, 128), ("dn%d_1" % m, 22, 128)]
    return pl


PLAN = block_plan()
WPL = sum(k * n for _, k, n in PLAN)


def _pk(blk):
    K = blk.shape[0] // 128
    n = blk.shape[1]
    return blk.reshape(K, 128, n).transpose(1, 0, 2).reshape(128, K * n)


def host_pack_weights(inp, nl):
    out = np.empty((128, WPL * nl), np.float32)
    z = np.zeros
    for l in range(nl):
        Wl = inp["w_in"][l]
        vd = inp["vres_down"][l - 1] if l > 0 else z((D, 32), np.float32)
        blocks = []
        blocks.append(Wl[:, 3072:3328])
        blocks.append(np.concatenate([Wl[:, 3328:3360], vd, z((D, 64), np.float32), Wl[:, 6432:6448]], axis=1))
        w_up = inp["rwkv_w_up"][l]
        a_up = inp["rwkv_a_up"][l]
        g_up = inp["rwkv_g_up"][l]
        v_up = inp["vres_up"][l - 1] if l > 0 else z((32, 1024), np.float32)
        wpad = np.concatenate([w_up, z((64, 1024), np.float32)], axis=0)
        apad = np.concatenate([z((64, 1024), np.float32), a_up], axis=0)
        g1 = g_up[0:128]
        g2 = np.concatenate([g_up[128:160], z((96, 1024), np.float32)], axis=0)
        vpad = np.concatenate([z((32, 1024), np.float32), v_up, z((64, 1024), np.float32)], axis=0)
        for p in range(8):
            s = slice(p * 128, (p + 1) * 128)
            blocks.append(np.concatenate([Wl[:, p * 128:(p + 1) * 128], Wl[:, 1024 + p * 128:1024 + (p + 1) * 128]], axis=1))
            blocks.append(Wl[:, 2048 + p * 128:2048 + (p + 1) * 128])
            blocks.append(np.concatenate([wpad[:, s], apad[:, s], g1[:, s], g2[:, s], vpad[:, s]], axis=1))
        for j in range(4):
            blocks.append(np.concatenate([Wl[:, GB + j * 128:GB + (j + 1) * 128],
                                          Wl[:, GB + 512 + j * 128:GB + 512 + (j + 1) * 128]], axis=1))
            blocks.append(Wl[:, GB + 1024 + 2 * j * 128:GB + 1024 + (2 * j + 2) * 128])
            blocks.append(Wl[:, GB + 2048 + 2 * j * 128:GB + 2048 + (2 * j + 2) * 128])
        Wo = inp["w_out"][l]
        for i in range(8):
            blocks.append(Wo[:, 256 * i:256 * (i + 1)])
        Wg = inp["ffn_w_gate"][l]
        Wu = inp["ffn_w_up"][l]
        for f in range(44):
            blocks.append(np.concatenate([Wg[:, 128 * f:128 * (f + 1)], Wu[:, 128 * f:128 * (f + 1)]], axis=1))
        Wd = inp["ffn_w_down"][l]
        for m in range(16):
            for h in range(2):
                blocks.append(Wd[h * 2816:(h + 1) * 2816, 128 * m:128 * (m + 1)])
        off = l * WPL
        assert len(blocks) == len(PLAN)
        for (nm, K, n), b in zip(PLAN, blocks):
            assert b.shape == (K * 128, n), (nm, b.shape)
            out[:, off:off + K * n] = _pk(np.ascontiguousarray(b, dtype=np.float32))
            off += K * n
        assert off == (l + 1) * WPL
    return out


def host_pack_params(inp, nl):
    pp = np.zeros((128, NPP), np.float32)

    def colv(v, n):
        return np.asarray(v, np.float32).reshape(n, 128).T

    for l in range(nl):
        b = l * NPL

        def put(name, arr):
            pp[:, b + POFF[name]:b + POFF[name] + arr.shape[1]] = arr

        put("anw", colv(inp["attn_norm_w"][l], 16))
        put("fnw", colv(inp["ffn_norm_w"][l], 16))
        mu = np.asarray(inp["rwkv_mu"][l], np.float32)
        put("mu_r", colv(mu[0:1024], 8))
        put("mu_k", colv(mu[1024:2048], 8))
        put("mu_v", colv(mu[2048:3072], 8))
        put("mu_A", colv(mu[3072:3200], 1))
        put("mu_B", colv(mu[3200:3328], 1))
        vm = np.asarray(inp["vres_mu"][l - 1], np.float32) if l > 0 else np.zeros(32, np.float32)
        put("mu_C", colv(np.concatenate([mu[3328:3360], vm, np.zeros(64, np.float32)]), 1))
        put("w0", colv(inp["rwkv_w0"][l], 8))
        put("a0", colv(inp["rwkv_a0"][l], 8))
        put("k_k", colv(inp["rwkv_k_k"][l], 8))
        put("k_a", colv(inp["rwkv_k_a"][l], 8))
        put("r_k", colv(np.asarray(inp["rwkv_r_k"][l]).reshape(-1), 8))
        put("ln_w", colv(inp["rwkv_ln_w"][l], 8))
        put("ln_b", colv(inp["rwkv_ln_b"][l], 8))
        if l > 0:
            put("v0", colv(inp["vres_v0"][l - 1], 8))
        cw = np.asarray(inp["gdn_conv_w"][l], np.float32)
        put("conv", cw.reshape(4, 16, 128).transpose(2, 1, 0).reshape(128, 64))
        put("gnw", colv(inp["gdn_norm_w"][l], 1))
        put("Alog", np.tile(np.asarray(inp["gdn_A_log"][l], np.float32)[None, :], (128, 1)))
        put("dtb", np.tile(np.asarray(inp["gdn_dt_bias"][l], np.float32)[None, :], (128, 1)))
    pp[:, NPL * NLAYER:NPL * NLAYER + 16] = colv(inp["final_norm_w"], 16)
    return pp


def build_program(NT, NL, dbg=None):
    nc = bass.Bass("TRN2", target_bir_lowering=False)
    Ttot = NT * TT
    xT_d = nc.dram_tensor("xT", [D, Ttot], F32, kind="ExternalInput").ap()
    wp_d = nc.dram_tensor("wp", [128, WPL * NL], F32, kind="ExternalInput").ap()
    pp_d = nc.dram_tensor("pp", [128, NPP], F32, kind="ExternalInput").ap()
    oT_d = nc.dram_tensor("oT", [D, Ttot], F32, kind="ExternalOutput").ap()
    dbg_d = {}
    if dbg:
        for nm, shp in dbg.items():
            dbg_d[nm] = nc.dram_tensor("dbg_" + nm, list(shp), F32, kind="ExternalOutput").ap()
    xT_v = xT_d.rearrange("(c p) t -> p c t", p=128)
    oT_v = oT_d.rearrange("(c p) t -> p c t", p=128)

    P = Prog(nc)
    S, PS = P.sbuf, P.psum
    xT = S("xT_sb", [128, 16, TT], F32)
    hT = S("hT", [128, 16, TT], BF16)
    scr = S("scr", [128, SCRN], BF16)
    stage = [S("stage%d" % i, [128, UNIT if not DMACAST else 8], F32) for i in range(2)]
    wb = [S("wb%d" % i, [128, 4096], BF16) for i in range(NWB)]
    pp = S("pp_sb", [128, NPP], F32)
    der = S("der", [128, NLAYER * 24], F32)
    Srw = [S("Srw%d" % l, [128, 8, 128], F32) for l in range(NL)]
    Sgd = [S("Sgd%d" % l, [128, 8, 128], F32) for l in range(NL)]
    vfirst = S("vfirst", [128, 8, TT], BF16)
    carry = [S("carry%d" % l, [128, 27 + 48], F32) for l in range(NL)]
    identb = S("identb", [128, 128], BF16)
    identf = S("identf", [128, 128], F32)
    onesb = S("onesb", [128, 128], BF16)
    onesf = S("onesf", [128, 128], F32)
    bdones = S("bdones", [128, 128], BF16)
    UTs = S("UTs", [128, 128], F32)
    UTi = S("UTi", [128, 128], F32)
    LTs = S("LTs", [128, 128], F32)
    BIGU = S("BIGU", [128, 128], F32)
    NBIGL = S("NBIGL", [128, 128], F32)
    scanmask = S("scanmask", [128, TT], F32)
    pbank = [PS("pb%d" % i, [128, 512], F32) for i in range(8)]
    pdq = {"i": 0}

    def PD():
        t = pbank[pdq["i"] % 5]
        pdq["i"] += 1
        return t

    pquart = []
    for i in range(5, 8):
        for q in range(4):
            pquart.append(QView(pbank[i], pbank[i].h[:, q * 128:(q + 1) * 128]))
    pqq = {"i": 0}

    def PQ():
        t = pquart[pqq["i"] % 12]
        pqq["i"] += 1
        return t

    f32s = [S("f%d" % i, [128, TT + 4], F32) for i in range(12)]
    yT = Tk(scr.h[:, 0:16 * TT].rearrange("p (c t) -> p c t", t=TT), "yT")
    actT = Tk(scr.h[:, 0:44 * TT].rearrange("p (c t) -> p c t", t=TT), "actT")
    _bx = {"o": 16 * TT}
    mixer_tiles = [yT]

    def BX(name, n):
        o = _bx["o"]
        _bx["o"] += n
        assert _bx["o"] <= SCRN, name
        t = Tk(scr.h[:, o:o + n], name)
        mixer_tiles.append(t)
        return t

    sqb = BX("sqb", TT)
    actA = BX("actA", TT)
    actB = BX("actB", TT)
    actC = BX("actC", TT)
    bd_a = BX("bd_a", 1024)
    bd_r = BX("bd_r", 1024)
    bd_b = BX("bd_b", 1024)
    bd_k = BX("bd_k", 1024)
    bd_v = BX("bd_v", 1024)
    tr_b = BX("tr_b", 1024)
    tr_k = BX("tr_k", 1024)
    tr_v = BX("tr_v", 1024)
    NBt = {nm: BX("nb_" + nm, 512) for nm in ("N", "B", "n0", "n1", "b0", "b1", "X0", "X1", "G1", "G2", "G3", "G4", "G5", "G6")}
    sm = [BX("sm%d" % i, 128) for i in range(4)]
    qTb = BX("qTb", TT)
    kTb = BX("kTb", TT)
    ybf = sqb
    abtm = S("abtm", [128, 4, 16], F32)
    gsc = {nm: S("gsc_" + nm, [128, 4, 8], F32) for nm in
           ("g", "beta", "nbeta", "gc", "ngc", "egc", "begc", "ekg", "egl", "tmp", "tmp2")}
    wcs = S("wcs", [128, 8], F32)
    gf = [S("gf%d" % i, [128, 128], F32) for i in range(4)]

    M = mybir.AluOpType
    P.memset(onesf[:, :], 1.0, eng="pool")
    P.memset(identf[:, :], 1.0, eng="pool")
    P.memset(UTs[:, :], 1.0, eng="pool")
    P.memset(UTi[:, :], 1.0, eng="pool")
    P.memset(LTs[:, :], 1.0, eng="pool")

    def asel(t, pat, cmp, base, cm):
        P.op("pool", lambda e: e.affine_select(t.h[:, :], t.h[:, :], pat, cmp, 0.0, base=base, channel_multiplier=cm),
             R=[t], W=[t])

    asel(identf, [[-1, 128]], M.is_equal, 0, 1)
    asel(UTs, [[1, 128]], M.is_ge, -1, -1)
    asel(UTi, [[1, 128]], M.is_ge, 0, -1)
    asel(LTs, [[-1, 128]], M.is_ge, -1, 1)
    P.copy(identb[:, :], identf[:, :])
    P.copy(onesb[:, :], onesf[:, :])
    P.memset(bdones[:, :], 0.0)
    P.memset(bdones[0:64, 0:64], 1.0)
    P.memset(bdones[64:128, 64:128], 1.0)
    P.ts(BIGU[:, :], UTi[:, :], BIG, M.mult)
    P.ts(NBIGL[:, :], LTs[:, :], -BIG, M.mult)
    P.memset(scanmask[:, :], 1.0)
    P.op("dve", lambda e: e.memset(scanmask.h[:, 0:TT:64], 0.0), R=[], W=[scanmask])
    for l in range(NL):
        P.memset(Srw[l][:, :, :], 0.0, eng="pool")
        P.memset(Sgd[l][:, :, :], 0.0, eng="pool")
        P.memset(carry[l][:, :], 0.0, eng="pool")
    for t_ in (bd_a, bd_r, bd_b, bd_k, bd_v):
        P.memset(t_[:, :], 0.0, eng="pool")
    P.dma("sp", pp[:, :], pp_d[:, :], "pp")
    for l in range(NL):
        b = l * NPL
        d = l * 24
        P.ts(der[:, d:d + 8], pp[:, b + POFF["w0"]:b + POFF["w0"] + 8], -1.0, M.mult)
        P.ts(der[:, d + 8:d + 16], pp[:, b + POFF["k_a"]:b + POFF["k_a"] + 8], -1.0, M.mult, 1.0, M.add)
        P.act(der[:, d + 16:d + 24], pp[:, b + POFF["Alog"]:b + POFF["Alog"] + 8], AF.Exp)
        P.ts(der[:, d + 16:d + 24], der[:, d + 16:d + 24], -1.0, M.mult)

    wst = {"off": 0, "unit": 0, "blk": 0, "planidx": 0}

    def next_block(l, name):
        nm, K, n = PLAN[wst["planidx"] % len(PLAN)]
        assert nm == name, (nm, name)
        wst["planidx"] += 1
        tot = K * n
        slot = wb[wst["blk"] % NWB]
        wst["blk"] += 1
        if DMACAST:
            sname = "wb%d" % ((wst["blk"] - 1) % NWB)
            P.op("pool", lambda e, slot=slot, tot=tot, off=wst["off"]: e.dma_start(out=slot.h[:, 0:tot], in_=wp_d[:, off:off + tot], max_dma_last_dim=4096),
                 R=[], W=[slot], dma_sem=sname)
            wst["off"] += tot
        else:
            o = 0
            while o < tot:
                u = min(UNIT, tot - o)
                st = stage[wst["unit"] % 2]
                wst["unit"] += 1
                P.dma("sp", st[:, 0:u], wp_d[:, wst["off"]:wst["off"] + u], "st%d" % ((wst["unit"] - 1) % 2))
                if name.startswith(("out", "gu", "dn")):
                    ceng = ("dve", "act", "dve", "act", "pool")[wst["unit"] % 5]
                else:
                    ceng = "pool"
                P.copy(slot[:, o:o + u], st[:, 0:u], eng=ceng)
                wst["off"] += u
                o += u

        def view(kc, c0, c1):
            return V(slot, slot.h[:, kc * n + c0:kc * n + c1])

        return view

    def dense(view, m, rhs_fn, K=16, ps=None, start=True, stop=True):
        if ps is None:
            ps = PD()
        for kc in range(K):
            P.mm(ps[:, :], view(kc, m * 128, (m + 1) * 128), rhs_fn(kc), start=(start and kc == 0), stop=(stop and kc == K - 1))
        return ps

    def pcol(l, name, i=0):
        o = l * NPL + POFF[name] + i
        return pp[:, o:o + 1]

    def rmsnorm_to_hT(wname, l):
        ps = PD()
        for c in range(16):
            P.act(hT[:, c, :], xT[:, c, :], AF.Square)
        for c in range(16):
            P.mm(ps[:, :], onesb[:, :], hT[:, c, :], start=(c == 0), stop=(c == 15))
        rstd = f32s[11]
        P.act(rstd[:, 0:TT], ps[:, :], AF.Sqrt, bias=1e-5, scale=1.0 / D)
        P.op("dve", lambda e: e.reciprocal(rstd.h[:, 0:TT], rstd.h[:, 0:TT]), R=[rstd], W=[rstd])
        for c in range(16):
            if wname == "finw":
                sc = pp[:, NPL * NLAYER + c:NPL * NLAYER + c + 1]
            else:
                sc = pcol(l, wname, c)
            P.stt(hT[:, c, :], xT[:, c, :], sc, rstd[:, 0:TT], M.mult, M.mult)
        return rstd

    def lerp(ps, l, cidx, mu, raw, dtmp, out):
        P.copy(raw[:, 0:1], carry[l][:, cidx:cidx + 1], eng="act")
        P.act(raw[:, 1:TT + 1], ps[:, :], AF.Copy)
        P.copy(carry[l][:, cidx:cidx + 1], raw[:, TT:TT + 1], eng="act")
        P.tt(dtmp[:, 0:TT], raw[:, 0:TT], raw[:, 1:TT + 1], M.subtract)
        P.stt(out, dtmp[:, 0:TT], mu, raw[:, 1:TT + 1], M.mult, M.add)

    def rsqrt_inplace(t, ps, scale, eps):
        P.act(t, ps, AF.Sqrt, bias=eps, scale=scale)
        P.op("dve", lambda e: e.reciprocal(t.ap, t.ap), R=[t], W=[t])

    def q4(t, i):
        return V(t, t.h[:, i * 128:(i + 1) * 128]) if isinstance(t, Tk) else t[:, i * 128:(i + 1) * 128]

    def neumann_b(levels):
        Nm, Bm = NBt["N"], NBt["B"]
        X, Xo = NBt["X0"], NBt["X1"]
        for i in range(4):
            P.tt(q4(X, i), q4(Bm, i), identb[:, :], M.add)
        Pn, Pb = Nm, Bm
        nn, bb = [NBt["n0"], NBt["n1"]], [NBt["b0"], NBt["b1"]]
        for lv in range(levels):
            q1 = PD()
            for i in range(4):
                P.mm(q4(q1, i), q4(Pb, i), q4(Pn, i))
            Pn2 = nn[lv % 2]
            P.copy(Pn2[:, :], q1[:, :], eng="act")
            if lv < levels - 1:
                q2 = PD()
                for i in range(4):
                    P.mm(q4(q2, i), q4(Pn, i), q4(Pb, i))
                Pb2 = bb[lv % 2]
                P.copy(Pb2[:, :], q2[:, :], eng="dve")
            else:
                Pb2 = None
            q3 = PD()
            for i in range(4):
                P.mm(q4(q3, i), q4(Pn2, i), q4(X, i), start=True, stop=False)
                P.mm(q4(q3, i), identb[:, :], q4(X, i), start=False, stop=True)
            P.copy(Xo[:, :], q3[:, :], eng=("dve" if lv % 2 else "act"))
            X, Xo = Xo, X
            Pn, Pb = Pn2, Pb2
        return X

    def dump(name, v):
        if dbg and name in dbg_d:
            tok = P.dma("sp", dbg_d[name], v, "dbg", out_is_dram=True, in_is_dram=False)

    def layer(l, ti):
        d0 = l * 24
        if LIMIT == 0:
            return
        wst["off"] = l * WPL
        wst["planidx"] = 0
        P.fence([actT], mixer_tiles)
        for t_ in (bd_a, bd_r, bd_b, bd_k, bd_v):
            P.memset(t_[:, :], 0.0, eng="pool")
        rmsnorm_to_hT("anw", l)
        hk = lambda kc: hT[:, kc, :]
        vw = next_block(l, "lab")
        psA = dense(vw, 0, hk)
        raw, dtmp = f32s[0], f32s[1]
        lerp(psA, l, 24, pcol(l, "mu_A"), raw, dtmp, dtmp[:, 0:TT])
        P.act(actA[0:64, :], dtmp[0:64, 0:TT], AF.Tanh)
        P.copy(actA[64:128, :], dtmp[64:128, 0:TT], eng="dve")
        psB = dense(vw, 1, hk)
        lerp(psB, l, 25, pcol(l, "mu_B"), raw, dtmp, dtmp[:, 0:TT])
        P.act(actB[:, :], dtmp[:, 0:TT], AF.Sigmoid)
        vw = next_block(l, "lc")
        psC = dense(vw, 0, hk)
        lerp(psC, l, 26, pcol(l, "mu_C"), raw, dtmp, dtmp[:, 0:TT])
        P.act(actC[0:32, :], dtmp[0:32, 0:TT], AF.Sigmoid)
        P.copy(actC[32:64, :], dtmp[32:64, 0:TT], eng="dve")
        P.copy(actC[64:128, :], dtmp[64:128, 0:TT], eng="dve")
        for s4 in range(4):
            q = PQ()
            for kc in range(16):
                P.mm(q[:, 0:16], hT[:, kc, s4 * 128:(s4 + 1) * 128], vw(kc, 128, 144), start=(kc == 0), stop=(kc == 15))
            P.copy(abtm[:, s4, :], q[:, 0:16], eng="act")
        if LIMIT == 1:
            return
        for p in range(8):
            rwkv_pair(l, ti, p, hk)
            if LIMIT == 2:
                return
        if LIMIT == 3:
            return
        gdn_scalars(l)
        if LIMIT == 4:
            return
        for j in range(4):
            gdn_group(l, ti, j, hk)
            if LIMIT == 5:
                return
        if LIMIT == 6:
            return
        for i in range(8):
            vw = next_block(l, "out%d" % i)
            for mm_ in range(2):
                ps = dense(vw, mm_, lambda kc: yT[:, kc, :])
                m = 2 * i + mm_
                P.tt(xT[:, m, :], ps[:, :], xT[:, m, :], M.add)
        P.fence(mixer_tiles, [actT])
        rmsnorm_to_hT("fnw", l)
        for f in range(44):
            vw = next_block(l, "gu%d" % f)
            pg = dense(vw, 0, hk)
            sg = f32s[f % 2]
            P.act(sg[:, 0:TT], pg[:, :], AF.Silu)
            pu = dense(vw, 1, hk)
            P.tt(actT[:, f, :], pu[:, :], sg[:, 0:TT], M.mult)
        for m in range(16):
            ps = PD()
            for h in range(2):
                vw = next_block(l, "dn%d_%d" % (m, h))
                for kk in range(22):
                    P.mm(ps[:, :], vw(kk, 0, 128), actT[:, h * 22 + kk, :], start=(h == 0 and kk == 0), stop=(h == 1 and kk == 21))
            P.tt(xT[:, m, :], ps[:, :], xT[:, m, :], M.add)

    def bdwrite(dst, fn):
        dv = dst.h[:, :].rearrange("p (c s) -> p c s", s=128)
        for h in range(2):
            rows = slice(h * 64, (h + 1) * 64)
            fn(rows, V(dst, dv[rows, :, h * 64:(h + 1) * 64]))

    def c3(t, rows):
        return V(t, t.h[rows, 0:TT].rearrange("p (c s) -> p c s", s=64))

    def rwkv_pair(l, ti, p, hk):
        d0 = l * 24
        raw, dtmp, Rt, Kt, Vt, ewp, cumn, Et, kk, bvec, asig, kmod = f32s[0:12]
        vw = next_block(l, "rk%d" % p)
        ps = dense(vw, 0, hk)
        lerp(ps, l, p, pcol(l, "mu_r", p), raw, dtmp, Rt[:, 0:TT])
        ps = dense(vw, 1, hk)
        lerp(ps, l, 8 + p, pcol(l, "mu_k", p), raw, dtmp, Kt[:, 0:TT])
        vw = next_block(l, "v%d" % p)
        ps = dense(vw, 0, hk)
        lerp(ps, l, 16 + p, pcol(l, "mu_v", p), raw, dtmp, Vt[:, 0:TT])
        up = next_block(l, "up%d" % p)
        pw = PD()
        P.mm(pw[:, :], up(0, 0, 128), actA[:, :])
        P.act(ewp[:, 0:TT], pw[:, :], AF.Exp, bias=der[:, d0 + p:d0 + p + 1], scale=-1.0)
        P.act(ewp[:, 0:TT], ewp[:, 0:TT], AF.Ln, bias=1.0)
        P.act(ewp[:, 0:TT], ewp[:, 0:TT], AF.Exp, bias=-0.5, scale=-1.0)
        P.op("dve", lambda e: e.tensor_tensor_scan(cumn.h[:, 0:TT], scanmask.h[:, :], ewp.h[:, 0:TT], 0.0, M.mult, M.add),
             R=[scanmask, ewp], W=[cumn])
        P.act(wcs[:, :], cumn[:, 63:TT:64], AF.Exp, scale=-1.0)
        if SUBLIM == 1:
            return
        pa = PD()
        P.mm(pa[:, :], up(0, 128, 256), actA[:, :])
        P.act(asig[:, 0:TT], pa[:, :], AF.Sigmoid, bias=pcol(l, "a0", p))
        P.ts(kk[:, 0:TT], Kt[:, 0:TT], pcol(l, "k_k", p), M.mult)
        P.act(sqb[:, :], kk[:, 0:TT], AF.Square)
        pss = PD()
        P.mm(pss[:, :], bdones[:, :], sqb[:, :])
        rsqrt_inplace(bvec[:, 0:TT], pss[:, :], 1.0, 1e-12)
        P.tt(kk[:, 0:TT], kk[:, 0:TT], bvec[:, 0:TT], M.mult)
        P.tt(bvec[:, 0:TT], kk[:, 0:TT], asig[:, 0:TT], M.mult)
        P.ts(kmod[:, 0:TT], asig[:, 0:TT], pcol(l, "k_a", p), M.mult, der[:, d0 + 8 + p:d0 + 9 + p], M.add)
        P.tt(kmod[:, 0:TT], kmod[:, 0:TT], Kt[:, 0:TT], M.mult)
        if l == 0:
            P.copy(vfirst[:, p, :], Vt[:, 0:TT], eng="act")
        else:
            pv = PD()
            P.mm(pv[:, :], up(0, 512, 640), actC[:, :])
            P.act(dtmp[:, 0:TT], pv[:, :], AF.Sigmoid, bias=pcol(l, "v0", p))
            P.copy(raw[:, 0:TT], vfirst[:, p, :], eng="act")
            P.tt(raw[:, 0:TT], raw[:, 0:TT], Vt[:, 0:TT], M.subtract)
            P.tt(raw[:, 0:TT], raw[:, 0:TT], dtmp[:, 0:TT], M.mult)
            P.tt(Vt[:, 0:TT], Vt[:, 0:TT], raw[:, 0:TT], M.add)
        pg = PD()
        P.mm(pg[:, :], up(0, 256, 384), actB[:, :], start=True, stop=False)
        P.mm(pg[:, :], up(0, 384, 512), actC[:, :], start=False, stop=True)
        gate = Kt
        P.act(gate[:, 0:TT], pg[:, :], AF.Copy)
        P.stt(sqb[:, :], Rt[:, 0:TT], pcol(l, "r_k", p), kmod[:, 0:TT], M.mult, M.mult)
        pbn = PD()
        P.mm(pbn[:, :], bdones[:, :], sqb[:, :])
        bonus = asig
        P.tt(bonus[:, 0:TT], pbn[:, :], Vt[:, 0:TT], M.mult)
        if SUBLIM == 2:
            return
        P.tt(Et[:, 0:TT], ewp[:, 0:TT], cumn[:, 0:TT], M.subtract)
        P.act(Et[:, 0:TT], Et[:, 0:TT], AF.Exp)
        bdwrite(bd_a, lambda rows, o: P.stt(o, c3(kk, rows), -1.0, c3(Et, rows), M.mult, M.mult))
        P.act(Et[:, 0:TT], cumn[:, 0:TT], AF.Exp, scale=-1.0)
        bdwrite(bd_r, lambda rows, o: P.tt(o, c3(Rt, rows), c3(Et, rows), M.mult))
        P.act(Et[:, 0:TT], cumn[:, 0:TT], AF.Exp)
        bdwrite(bd_b, lambda rows, o: P.tt(o, c3(bvec, rows), c3(Et, rows), M.mult))
        bdwrite(bd_k, lambda rows, o: P.tt(o, c3(kmod, rows), c3(Et, rows), M.mult))
        bdwrite(bd_v, lambda rows, o: P.copy(o, c3(Vt, rows), eng="act"))
        if SUBLIM == 3:
            return
        yout = raw
        S_ = Srw[l]
        Sb, RHS, U = sm[0], sm[1], sm[2]
        for half in range(2):
            cb = half * 4
            hs = slice(cb * 128, (cb + 4) * 128)
            bdc = lambda t, i: V(t, t.h[:, (cb + i) * 128:(cb + i + 1) * 128])
            for src, dst in ((bd_b, tr_b), (bd_k, tr_k), (bd_v, tr_v)):
                q = PD()
                for i in range(4):
                    P.mm(q4(q, i), bdc(src, i), identb[:, :])
                P.copy(dst[:, hs], q[:, :], eng="act")
            for lhs, rhs, msk, dst in ((bd_a, bd_b, LTs, NBt["N"]), (bd_b, bd_a, UTs, NBt["B"]), (bd_k, bd_a, UTs, NBt["G1"]),
                                       (bd_b, bd_r, UTi, NBt["G2"]), (bd_k, bd_r, UTi, NBt["G3"])):
                q = PD()
                for i in range(4):
                    P.mm(q4(q, i), bdc(lhs, i), bdc(rhs, i))
                for i in range(4):
                    P.tt(q4(dst, i), q4(q, i), msk[:, :], M.mult)
            X = neumann_b(5)
            AkT, ArbT, ArkT = NBt["G1"], NBt["G2"], NBt["G3"]
            for i in range(4):
                c = cb + i
                cs = slice(c * 128, (c + 1) * 128)
                A_, R_ = bdc(bd_a, i), bdc(bd_r, i)
                P.copy(Sb[:, :], S_[:, p, :], eng="act")
                q = PQ()
                P.mm(q[:, :], A_, Sb[:, :], start=True, stop=False)
                P.mm(q[:, :], q4(AkT, i), tr_v[:, cs], start=False, stop=True)
                P.copy(RHS[:, :], q[:, :], eng="act")
                q = PQ()
                P.mm(q[:, :], q4(X, i), RHS[:, :])
                P.copy(U[:, :], q[:, :], eng="dve")
                q = PQ()
                P.mm(q[:, :], Sb[:, :], R_, start=True, stop=False)
                P.mm(q[:, :], U[:, :], q4(ArbT, i), start=False, stop=False)
                P.mm(q[:, :], tr_v[:, cs], q4(ArkT, i), start=False, stop=True)
                P.copy(yout[0:64, c * 64:(c + 1) * 64], q[0:64, 0:64], eng="act")
                P.copy(yout[64:128, c * 64:(c + 1) * 64], q[64:128, 64:128], eng="dve")
                q = PQ()
                P.mm(q[:, :], tr_b[:, cs], U[:, :], start=True, stop=False)
                P.mm(q[:, :], tr_k[:, cs], tr_v[:, cs], start=False, stop=True)
                P.tt(S_[:, p, :], q[:, :], S_[:, p, :], M.add)
                P.ts(S_[:, p, :], S_[:, p, :], wcs[:, c:c + 1], M.mult)
        P.copy(ybf[:, :], yout[:, 0:TT], eng="act")
        pm = PD()
        P.mm(pm[:, :], bdones[:, :], ybf[:, :])
        yc = dtmp
        P.stt(yc[:, 0:TT], pm[:, :], -1.0 / 64, yout[:, 0:TT], M.mult, M.add)
        P.act(sqb[:, :], yc[:, 0:TT], AF.Square)
        pv2 = PD()
        P.mm(pv2[:, :], bdones[:, :], sqb[:, :])
        rsqrt_inplace(ewp[:, 0:TT], pv2[:, :], 1.0 / 64, 64e-5)
        P.tt(yc[:, 0:TT], yc[:, 0:TT], ewp[:, 0:TT], M.mult)
        P.ts(yc[:, 0:TT], yc[:, 0:TT], pcol(l, "ln_w", p), M.mult, pcol(l, "ln_b", p), M.add)
        P.tt(yc[:, 0:TT], yc[:, 0:TT], bonus[:, 0:TT], M.add)
        P.tt(yT[:, p, :], yc[:, 0:TT], gate[:, 0:TT], M.mult)

    def gdn_scalars(l):
        d0 = l * 24
        b = l * NPL
        G = gsc
        for c in range(4):
            P.tt(G["tmp"][:, c, :], abtm[:, c, 0:8], pp[:, b + POFF["dtb"]:b + POFF["dtb"] + 8], M.add)
            P.act(G["tmp"][:, c, :], G["tmp"][:, c, :], AF.Exp)
            P.act(G["tmp"][:, c, :], G["tmp"][:, c, :], AF.Ln, bias=1.0)
            P.tt(G["g"][:, c, :], G["tmp"][:, c, :], der[:, d0 + 16:d0 + 24], M.mult)
            P.act(G["beta"][:, c, :], abtm[:, c, 8:16], AF.Sigmoid)
            P.ts(G["nbeta"][:, c, :], G["beta"][:, c, :], -1.0, M.mult)
            q = PQ()
            P.mm(q[:, 0:8], UTi[:, :], G["g"][:, c, :])
            P.copy(G["gc"][:, c, :], q[:, 0:8], eng="act")
            P.ts(G["ngc"][:, c, :], G["gc"][:, c, :], -1.0, M.mult)
            P.act(G["egc"][:, c, :], G["gc"][:, c, :], AF.Exp)
            P.tt(G["begc"][:, c, :], G["egc"][:, c, :], G["beta"][:, c, :], M.mult)
            q2 = PQ()
            P.mm(q2[:, 0:8], onesf[:, :], G["g"][:, c, :])
            P.act(G["egl"][:, c, :], q2[:, 0:8], AF.Exp)
            P.tt(G["tmp2"][:, c, :], q2[:, 0:8], G["gc"][:, c, :], M.subtract)
            P.act(G["ekg"][:, c, :], G["tmp2"][:, c, :], AF.Exp)

    def conv_silu(ps, l, cc, raw, acc, out, silu=True):
        co = 27 + cc * 3
        P.copy(raw[:, 0:3], carry[l][:, co:co + 3], eng="act")
        P.act(raw[:, 3:TT + 3], ps[:, :], AF.Copy)
        P.copy(carry[l][:, co:co + 3], raw[:, TT:TT + 3], eng="act")
        w = lambda tap: pcol(l, "conv", cc * 4 + tap)
        P.ts(acc[:, 0:TT], raw[:, 0:TT], w(0), M.mult)
        for tap in (1, 2, 3):
            P.stt(acc[:, 0:TT], raw[:, tap:TT + tap], w(tap), acc[:, 0:TT], M.mult, M.add)
        P.act(out, acc[:, 0:TT], AF.Silu)

    def gdn_group(l, ti, j, hk):
        G = gsc
        raw, acc, qf, kf, v0f, v1f, o0, o1, rn, t9 = f32s[0:10]
        zsf = [f32s[10], f32s[11]]
        vw = next_block(l, "gqk%d" % j)
        ps = dense(vw, 0, hk)
        conv_silu(ps, l, j, raw, acc, qf[:, 0:TT])
        ps = dense(vw, 1, hk)
        conv_silu(ps, l, 4 + j, raw, acc, kf[:, 0:TT])
        vw = next_block(l, "gv%d" % j)
        vf = [v0f, v1f]
        for hh in range(2):
            ps = dense(vw, hh, hk)
            conv_silu(ps, l, 8 + 2 * j + hh, raw, acc, vf[hh][:, 0:TT])
        vw = next_block(l, "gz%d" % j)
        for hh in range(2):
            ps = dense(vw, hh, hk)
            P.act(zsf[hh][:, 0:TT], ps[:, :], AF.Silu)
        for src, dst, sc in ((qf, qTb, 128.0 ** -0.5), (kf, kTb, 1.0)):
            P.act(sqb[:, :], src[:, 0:TT], AF.Square)
            pss = PD()
            P.mm(pss[:, :], onesb[:, :], sqb[:, :])
            rsqrt_inplace(rn[:, 0:TT], pss[:, :], 1.0, 1e-6)
            P.stt(src[:, 0:TT], src[:, 0:TT], sc, rn[:, 0:TT], M.mult, M.mult)
            P.copy(dst[:, :], src[:, 0:TT], eng="act")
        vTb = [tr_b, tr_k]
        for hh in range(2):
            P.copy(vTb[hh][:, 0:TT], vf[hh][:, 0:TT], eng="act")
        oT = [o0, o1]
        AqkT_, bv_, kbg_, kg_, qgT_, wTn_ = (NBt[k] for k in ("G1", "G2", "G3", "G4", "G5", "G6"))
        for hh in range(2):
            h = 2 * j + hh
            for c in range(4):
                cs = slice(c * 128, (c + 1) * 128)
                col = lambda nm: G[nm][:, c, h:h + 1]
                qkk = PQ()
                P.mm(qkk[:, :], kTb[:, cs], kTb[:, cs])
                KK = gf[0]
                P.copy(KK[:, :], qkk[:, :], eng="act")
                qqk = PQ()
                P.mm(qqk[:, :], kTb[:, cs], qTb[:, cs])
                QKT = gf[1]
                P.copy(QKT[:, :], qqk[:, :], eng="dve")
                qkt = PQ()
                P.mm(qkt[:, :], kTb[:, cs], identb[:, :])
                ktm = gf[2]
                P.copy(ktm[:, :], qkt[:, :], eng="act")
                gbc = gf[3]
                P.ts(gbc[:, :], onesf[:, :], col("g"), M.mult)
                q1 = PQ()
                P.mm(q1[:, :], gbc[:, :], UTi[:, :], start=True, stop=False)
                P.mm(q1[:, :], identf[:, :], BIGU[:, :], start=False, stop=True)
                Gs = t9
                P.act(Gs[:, 0:128], q1[:, :], AF.Exp, bias=col("gc"), scale=-1.0)
                P.stt(q4(NBt["N"], c), Gs[:, 0:128], col("nbeta"), KK[:, :], M.mult, M.mult)
                q2 = PQ()
                P.mm(q2[:, :], gbc[:, :], UTi[:, :], start=True, stop=False)
                P.mm(q2[:, :], identf[:, :], NBIGL[:, :], start=False, stop=True)
                P.act(Gs[:, 128:256], q2[:, :], AF.Exp, bias=col("ngc"), scale=1.0)
                P.tt(q4(AqkT_, c), QKT[:, :], Gs[:, 128:256], M.mult)
                q3 = PQ()
                P.mm(q3[:, :], q4(NBt["N"], c), identb[:, :])
                P.copy(q4(NBt["B"], c), q3[:, :], eng="act")
                q4_ = PQ()
                P.mm(q4_[:, :], vTb[hh][:, cs], identb[:, :])
                P.ts(q4(bv_, c), q4_[:, :], col("beta"), M.mult)
                P.ts(q4(kbg_, c), ktm[:, :], col("begc"), M.mult)
                P.ts(q4(kg_, c), ktm[:, :], col("ekg"), M.mult)
                q6 = PQ()
                P.mm(q6[:, :], gbc[:, :], UTi[:, :])
                P.act(Gs[:, 256:384], q6[:, :], AF.Exp)
                P.tt(q4(qgT_, c), qf[:, cs], Gs[:, 256:384], M.mult)
            X = neumann_b(6)
            q5 = PD()
            for c in range(4):
                P.mm(q4(q5, c), q4(kbg_, c), q4(X, c))
            P.act(wTn_[:, :], q5[:, :], AF.Copy, scale=-1.0)
            S_ = Sgd[l]
            Sb, vnew = sm[0], sm[3]
            for c in range(4):
                cs = slice(c * 128, (c + 1) * 128)
                col = lambda nm: G[nm][:, c, h:h + 1]
                P.copy(Sb[:, :], S_[:, h, :], eng="act")
                q7 = PQ()
                P.mm(q7[:, :], q4(X, c), q4(bv_, c), start=True, stop=False)
                P.mm(q7[:, :], q4(wTn_, c), Sb[:, :], start=False, stop=True)
                P.copy(vnew[:, :], q7[:, :], eng="dve")
                q8 = PQ()
                P.mm(q8[:, :], Sb[:, :], q4(qgT_, c), start=True, stop=False)
                P.mm(q8[:, :], vnew[:, :], q4(AqkT_, c), start=False, stop=True)
                P.copy(oT[hh][:, cs], q8[:, :], eng="act")
                q9 = PQ()
                P.mm(q9[:, :], q4(kg_, c), vnew[:, :])
                P.stt(S_[:, h, :], S_[:, h, :], col("egl"), q9[:, :], M.mult, M.add)
        for hh in range(2):
            h = 2 * j + hh
            P.act(sqb[:, :], oT[hh][:, 0:TT], AF.Square)
            pss = PD()
            P.mm(pss[:, :], onesb[:, :], sqb[:, :])
            rsqrt_inplace(rn[:, 0:TT], pss[:, :], 1.0 / 128, 1e-6)
            P.stt(oT[hh][:, 0:TT], oT[hh][:, 0:TT], pcol(l, "gnw"), rn[:, 0:TT], M.mult, M.mult)
            P.tt(yT[:, 8 + h, :], oT[hh][:, 0:TT], zsf[hh][:, 0:TT], M.mult)

    last_tok = None
    for ti in range(NT):
        ts_ = slice(ti * TT, (ti + 1) * TT)
        for c4 in range(4):
            P.dma("sp", xT[:, c4 * 4:(c4 + 1) * 4, :], xT_v[:, c4 * 4:(c4 + 1) * 4, ts_], "xin")
        for l in range(NL):
            layer(l, ti)
        ps = PD()
        for c in range(16):
            P.act(hT[:, c, :], xT[:, c, :], AF.Square)
        for c in range(16):
            P.mm(ps[:, :], onesb[:, :], hT[:, c, :], start=(c == 0), stop=(c == 15))
        rs = f32s[11]
        rsqrt_inplace(rs[:, 0:TT], ps[:, :], 1.0 / D, 1e-5)
        for c in range(16):
            o = NPL * NLAYER + c
            P.stt(xT[:, c, :], xT[:, c, :], pp[:, o:o + 1], rs[:, 0:TT], M.mult, M.mult)
        for c4 in range(4):
            last_tok = P.dma("sp", oT_v[:, c4 * 4:(c4 + 1) * 4, ts_], xT[:, c4 * 4:(c4 + 1) * 4, :], "xout", out_is_dram=True, in_is_dram=False)
    P.wait_tok("sp", last_tok)
    P.emit()
    return nc, P


_CACHE = {}


def run_cores(inp, NT, NL, ncores):
    wp = host_pack_weights(inp, NL)
    ppk = host_pack_params(inp, NL)
    x = np.asarray(inp["x"], np.float32)
    in_maps = []
    for b in range(ncores):
        in_maps.append({"xT": np.ascontiguousarray(x[b, :NT * TT, :].T), "wp": wp, "pp": ppk})
    nc, P = build_program(NT, NL)
    res = run_bass_kernel_spmd(nc, in_maps, core_ids=list(range(ncores)))
    outs = [np.ascontiguousarray(res.results[b]["oT"].T) for b in range(ncores)]
    return np.stack(outs, axis=0)


def kernel(**inputs):
    inp = {k: np.asarray(v) for k, v in inputs.items()}
    out = run_cores(inp, T_FULL // TT, NLAYER, 8)
    return out.astype(np.float32)
```
